# Optimizing a Trainium2 kernel written in Bass

```python
import numpy as np
import jax
import jax.numpy as jnp
from jax import lax

D_MODEL = 1024
BATCH = 4
SEQ = 8192
DEPTH = 2

GRID_W = 64
CTX_LEN = 256
HEAD_DIM = 64
ATTN_HEADS = 8
ATTN_KV_HEADS = 2
GROUP = ATTN_HEADS // ATTN_KV_HEADS
ATTN_DIM = ATTN_HEADS * HEAD_DIM
KV_DIM = ATTN_KV_HEADS * HEAD_DIM
RW_HEADS = 8
RW_DIM = RW_HEADS * HEAD_DIM
D_MIX = ATTN_DIM + RW_DIM
W_LORA = 64
A_LORA = 64
G_LORA = 128
CONV_W = 3
N_EXPERTS = 32
TOP_K = 4
D_EXPERT = D_MODEL
SWIGLU_LIMIT = 7.0
SWIGLU_ALPHA = 1.702
ROPE_THETA = 10000.0
ROPE_PAIRS = HEAD_DIM // 4
Q_BLOCK = 128
MOE_BLOCK = 256
NORM_EPS = 1e-6
GN_EPS = 64e-5
ATTN_SCALE = HEAD_DIM ** -0.5
SPLITS = (KV_DIM, KV_DIM, RW_DIM, RW_DIM, 2 * W_LORA, 2 * A_LORA, ATTN_DIM, RW_DIM, G_LORA)
STATE_COLS = 2 * KV_DIM + 2 * RW_DIM + 2 * W_LORA + 2 * A_LORA
IN_COLS = STATE_COLS + ATTN_DIM + RW_DIM + G_LORA

kernel_name = 'hybrid_gqa_rwkv7_moe_dit'


def rms_norm(x, g):
    xf = x.astype(jnp.float32)
    y = xf * lax.rsqrt(jnp.mean(xf * xf, axis=-1, keepdims=True) + NORM_EPS)
    return (y * g.astype(jnp.float32)).astype(x.dtype)


def heads(x, n):
    return x.reshape(x.shape[:-1] + (n, x.shape[-1] // n))


def split_cols(p, sizes):
    return jnp.split(p, np.cumsum(sizes)[:-1].tolist(), axis=-1)


def adaln(cvec, w_mod, b_mod):
    return jnp.split(jax.nn.silu(cvec) @ w_mod + b_mod, 6, axis=-1)


def modulate(h, shift, scale):
    return h * (1 + scale) + shift


def short_conv(u, w):
    T = u.shape[1]
    pad = CONV_W // 2
    up = jnp.pad(u, ((0, 0), (pad, pad), (0, 0)))
    return sum(up[:, i:i + T] * w[i] for i in range(CONV_W))


def axial_rope(rows):
    row = jnp.repeat(jnp.arange(rows), GRID_W)
    col = jnp.tile(jnp.arange(GRID_W), rows)
    pos = jnp.stack([row, col], axis=-1).astype(jnp.float32)
    freqs = ROPE_THETA ** (-jnp.arange(ROPE_PAIRS, dtype=jnp.float32) / ROPE_PAIRS)
    ang = pos[:, :, None] * freqs
    return jnp.cos(ang), jnp.sin(ang)


def apply_rope(x, cos, sin):
    B, T, H, Dh = x.shape
    xa = x.reshape(B, T, H, 2, 2, ROPE_PAIRS).astype(jnp.float32)
    x1, x2 = xa[..., 0, :], xa[..., 1, :]
    c, s = cos[None, :, None], sin[None, :, None]
    out = jnp.stack([x1 * c - x2 * s, x2 * c + x1 * s], axis=-2)
    return out.reshape(B, T, H, Dh).astype(x.dtype)


def gqa(q, k, v):
    B, Q = q.shape[:2]
    qg = q.reshape(B, Q, ATTN_KV_HEADS, GROUP, HEAD_DIM)
    s = jnp.einsum('bqhgd,bshd->bhgqs', qg, k, preferred_element_type=jnp.float32) * ATTN_SCALE
    p = jax.nn.softmax(s, axis=-1).astype(v.dtype)
    o = jnp.einsum('bhgqs,bshd->bqhgd', p, v)
    return o.reshape(B, Q, ATTN_DIM)


def latent_attention(q, k, v, kc, vc):
    B, T = q.shape[:2]
    k_all = jnp.concatenate([kc, k], axis=1)
    v_all = jnp.concatenate([vc, v], axis=1)
    nb = T // Q_BLOCK
    qb = jnp.moveaxis(q.reshape(B, nb, Q_BLOCK, ATTN_HEADS, HEAD_DIM), 1, 0)
    o = lax.map(lambda qi: gqa(qi, k_all, v_all), qb)
    return jnp.moveaxis(o, 0, 1).reshape(B, T, ATTN_DIM)


def rwkv_state_inputs(k_raw, v_raw, xw, xa, conv_w, w0, w_up, a0, a_up, k_k, k_a):
    B, T, _ = k_raw.shape
    k = short_conv(k_raw, conv_w[:, RW_DIM:2 * RW_DIM])
    v = short_conv(v_raw, conv_w[:, 2 * RW_DIM:])
    wl = w0 + jnp.einsum('btdl,dlc->btdc', jnp.tanh(xw.reshape(B, T, 2, W_LORA)), w_up)
    decay = jnp.exp(-jnp.exp(-jax.nn.softplus(-wl.astype(jnp.float32)) - 0.5))
    a = jax.nn.sigmoid(a0 + jnp.einsum('btdl,dlc->btdc', xa.reshape(B, T, 2, A_LORA), a_up))
    kk = heads(k * k_k, RW_HEADS).astype(jnp.float32)
    kk = kk * lax.rsqrt(jnp.maximum(jnp.sum(kk * kk, axis=-1, keepdims=True), 1e-24))
    k_dir = k[:, :, None, :] * (1 + (a - 1) * k_a)
    return (heads(k_dir, RW_HEADS), heads(v, RW_HEADS), heads(decay, RW_HEADS), heads(a, RW_HEADS), kk)


def wkv_scan(w, k, v, a_op, b_op, s0, r, reverse):
    xs = (w, k, v, a_op, b_op) + (() if r is None else (r,))
    xs = tuple(jnp.moveaxis(t.astype(jnp.float32), 1, 0) for t in xs)

    def step(S, xt):
        w_t, k_t, v_t, a_t, b_t = xt[:5]
        S = (S * w_t[:, :, None, :]
             + jnp.einsum('bhij,bhj->bhi', S, a_t)[..., None] * b_t[:, :, None, :]
             + v_t[..., None] * k_t[:, :, None, :])
        out = None if r is None else jnp.einsum('bhij,bhj->bhi', S, xt[5])
        return S, out

    S, out = lax.scan(step, s0, xs, reverse=reverse)
    return S, (None if r is None else jnp.moveaxis(out, 0, 1))


def scan_dir(st, d, s0, r, reverse):
    k_dir, v, decay, a, kk = st
    return wkv_scan(decay[:, :, d], k_dir[:, :, d], v, -kk, kk * a[:, :, d], s0, r, reverse)


def rwkv_readout(o_f, o_b, r, st, xg, g_up, r_k, ln_x_g, ln_x_b):
    k_dir, v = st[0], st[1]
    B, T = v.shape[:2]
    o = o_f + o_b
    mu = jnp.mean(o, axis=-1, keepdims=True)
    var = jnp.mean(jnp.square(o - mu), axis=-1, keepdims=True)
    gn = ((o - mu) * lax.rsqrt(var + GN_EPS)).reshape(B, T, RW_DIM) * ln_x_g + ln_x_b
    k_bonus = jnp.mean(k_dir.astype(jnp.float32), axis=2)
    bonus = jnp.sum(r.astype(jnp.float32) * k_bonus * r_k, axis=-1, keepdims=True) * v.astype(jnp.float32)
    g = jax.nn.sigmoid(xg) @ g_up
    return ((gn + bonus.reshape(B, T, RW_DIM)) * g).astype(v.dtype)


def token_mixer(n_lat, n_ctx, w_in, w_out, q_norm_g, k_norm_g, conv_w, w0, w_up, a0, a_up, g_up,
                k_k, k_a, r_k, ln_x_g, ln_x_b, cos, sin, last):
    B = n_lat.shape[0]
    k_at, v_at, k_rw, v_rw, xw, xa, q_at, r_rw, xg = split_cols(n_lat @ w_in, SPLITS)
    if last:
        ctx_parts = split_cols(n_ctx @ w_in[:, :STATE_COLS], SPLITS[:6])
    else:
        ctx_parts = split_cols(n_ctx @ w_in, SPLITS)
    ck_at, cv_at, ck_rw, cv_rw, cxw, cxa = ctx_parts[:6]

    q = apply_rope(rms_norm(heads(q_at, ATTN_HEADS), q_norm_g), cos, sin)
    k = apply_rope(rms_norm(heads(k_at, ATTN_KV_HEADS), k_norm_g), cos, sin)
    kc = rms_norm(heads(ck_at, ATTN_KV_HEADS), k_norm_g)
    vc = heads(cv_at, ATTN_KV_HEADS)
    o_at = latent_attention(q, k, heads(v_at, ATTN_KV_HEADS), kc, vc)

    rw = (conv_w, w0, w_up, a0, a_up, k_k, k_a)
    st_c = rwkv_state_inputs(ck_rw, cv_rw, cxw, cxa, *rw)
    st_l = rwkv_state_inputs(k_rw, v_rw, xw, xa, *rw)
    r = heads(short_conv(r_rw, conv_w[:, :RW_DIM]), RW_HEADS)
    r_c = None if last else heads(short_conv(ctx_parts[7], conv_w[:, :RW_DIM]), RW_HEADS)
    s0 = jnp.zeros((B, RW_HEADS, HEAD_DIM, HEAD_DIM), jnp.float32)
    s_cf, o_cf = scan_dir(st_c, 0, s0, r_c, False)
    s_cb, o_cb = scan_dir(st_c, 1, s0, r_c, True)
    _, o_lf = scan_dir(st_l, 0, s_cf, r, False)
    _, o_lb = scan_dir(st_l, 1, s_cb, r, True)
    o_rw = rwkv_readout(o_lf, o_lb, r, st_l, xg, g_up, r_k, ln_x_g, ln_x_b)

    y_lat = jnp.concatenate([o_at, o_rw.astype(o_at.dtype)], axis=-1) @ w_out
    if last:
        return y_lat, None
    q_c = rms_norm(heads(ctx_parts[6], ATTN_HEADS), q_norm_g)
    o_at_c = gqa(q_c, kc, vc)
    o_rw_c = rwkv_readout(o_cf, o_cb, r_c, st_c, ctx_parts[8], g_up, r_k, ln_x_g, ln_x_b)
    y_ctx = jnp.concatenate([o_at_c, o_rw_c.astype(o_at_c.dtype)], axis=-1) @ w_out
    return y_lat, y_ctx


def moe_ffn(h, router_w, router_b, e_w1, e_b1, e_w2, e_b2):
    N, D = h.shape
    logits = (h @ router_w + router_b).astype(jnp.float32)
    top_v, top_e = lax.top_k(logits, TOP_K)
    gates = jax.nn.softmax(top_v, axis=-1)
    flat_e = top_e.reshape(-1)
    flat_tok = jnp.arange(N * TOP_K, dtype=jnp.int32) // TOP_K
    flat_g = gates.reshape(-1)
    order = jnp.argsort(flat_e)
    e_sorted = flat_e[order]
    counts = jnp.bincount(flat_e, length=N_EXPERTS)
    padded = (counts + MOE_BLOCK - 1) // MOE_BLOCK * MOE_BLOCK
    start = jnp.cumsum(counts) - counts
    pstart = jnp.cumsum(padded) - padded
    dest = pstart[e_sorted] + jnp.arange(N * TOP_K, dtype=jnp.int32) - start[e_sorted]
    n_blocks = (N * TOP_K + MOE_BLOCK - 1) // MOE_BLOCK + N_EXPERTS
    rows = n_blocks * MOE_BLOCK
    slot_tok = jnp.full((rows,), N, jnp.int32).at[dest].set(flat_tok[order])
    slot_gate = jnp.zeros((rows,), jnp.float32).at[dest].set(flat_g[order])
    block_e = jnp.minimum(jnp.searchsorted(jnp.cumsum(padded), jnp.arange(n_blocks) * MOE_BLOCK, side='right'),
                          N_EXPERTS - 1)
    h_pad = jnp.concatenate([h, jnp.zeros((1, D), h.dtype)], axis=0)

    def run_block(args):
        toks, e = args
        u = h_pad[toks] @ e_w1[e] + e_b1[e]
        x_glu, x_lin = jnp.split(u, 2, axis=-1)
        x_glu = jnp.minimum(x_glu, SWIGLU_LIMIT)
        x_lin = jnp.clip(x_lin, -SWIGLU_LIMIT, SWIGLU_LIMIT)
        act = x_glu * jax.nn.sigmoid(SWIGLU_ALPHA * x_glu) * (x_lin + 1)
        return act @ e_w2[e] + e_b2[e]

    out = lax.map(run_block, (slot_tok.reshape(n_blocks, MOE_BLOCK), block_e)).reshape(rows, D)
    out = out * slot_gate[:, None].astype(out.dtype)
    y = jnp.zeros((N + 1, D), out.dtype).at[slot_tok].add(out)
    return y[:N]


def setup_inputs(seed: int = 0) -> dict:
    key = jax.random.key(seed)
    ks = jax.random.split(key, 30)
    L, D, E, F = DEPTH, D_MODEL, N_EXPERTS, D_EXPERT

    def nrm(k, shape, scale):
        return scale * jax.random.normal(k, shape, jnp.float32)

    return {
        'x': nrm(ks[0], (BATCH, SEQ, D), 1.0),
        'c': nrm(ks[1], (BATCH, D), 1.0),
        'ctx': nrm(ks[2], (BATCH, CTX_LEN, D), 1.0),
        'c_ctx': nrm(ks[3], (D,), 1.0),
        'norm_mix_g': 1.0 + nrm(ks[4], (L, D), 0.05),
        'norm_ffn_g': 1.0 + nrm(ks[5], (L, D), 0.05),
        'w_mod': nrm(ks[6], (L, D, 6 * D), 0.5 * D ** -0.5),
        'b_mod': nrm(ks[7], (L, 6 * D), 0.02),
        'w_in': nrm(ks[8], (L, D, IN_COLS), D ** -0.5),
        'w_out': nrm(ks[9], (L, D_MIX, D), D_MIX ** -0.5),
        'q_norm_g': 1.0 + nrm(ks[10], (L, HEAD_DIM), 0.05),
        'k_norm_g': 1.0 + nrm(ks[11], (L, HEAD_DIM), 0.05),
        'conv_w': nrm(ks[12], (L, CONV_W, 3 * RW_DIM), 0.1) + jax.nn.one_hot(CONV_W // 2, CONV_W)[None, :, None],
        'w0': jax.random.uniform(ks[13], (L, 2, RW_DIM), jnp.float32, -6.0, 1.0),
        'w_up': nrm(ks[14], (L, 2, W_LORA, RW_DIM), 0.5 * W_LORA ** -0.5),
        'a0': nrm(ks[15], (L, 2, RW_DIM), 0.5),
        'a_up': nrm(ks[16], (L, 2, A_LORA, RW_DIM), A_LORA ** -0.5),
        'g_up': nrm(ks[17], (L, G_LORA, RW_DIM), G_LORA ** -0.5),
        'k_k': 0.85 + nrm(ks[18], (L, RW_DIM), 0.05),
        'k_a': 1.0 + nrm(ks[19], (L, RW_DIM), 0.05),
        'r_k': nrm(ks[20], (L, RW_HEADS, HEAD_DIM), 0.1),
        'ln_x_g': 1.0 + nrm(ks[21], (L, RW_DIM), 0.05),
        'ln_x_b': nrm(ks[22], (L, RW_DIM), 0.02),
        'router_w': nrm(ks[23], (L, D, E), D ** -0.5),
        'router_b': nrm(ks[24], (L, E), 0.01),
        'e_w1': nrm(ks[25], (L, E, D, 2 * F), D ** -0.5),
        'e_b1': nrm(ks[26], (L, E, 2 * F), 0.01),
        'e_w2': nrm(ks[27], (L, E, F, D), F ** -0.5),
        'e_b2': nrm(ks[28], (L, E, D), 0.01),
        'norm_final_g': 1.0 + nrm(ks[29], (D,), 0.05),
    }


def reference(x, c, ctx, c_ctx, norm_mix_g, norm_ffn_g, w_mod, b_mod, w_in, w_out, q_norm_g, k_norm_g,
              conv_w, w0, w_up, a0, a_up, g_up, k_k, k_a, r_k, ln_x_g, ln_x_b,
              router_w, router_b, e_w1, e_b1, e_w2, e_b2, norm_final_g):
    B, T, D = x.shape
    C = ctx.shape[1]
    ROWS = T // GRID_W
    cos, sin = axial_rope(ROWS)
    x_lat, x_ctx = x, ctx
    for l in range(DEPTH):
        last = l == DEPTH - 1
        sh_m, sc_m, g_m, sh_f, sc_f, g_f = [p[:, None, :] for p in adaln(c, w_mod[l], b_mod[l])]
        csh_m, csc_m, cg_m, csh_f, csc_f, cg_f = adaln(c_ctx, w_mod[l], b_mod[l])
        y_lat, y_ctx = token_mixer(
            modulate(rms_norm(x_lat, norm_mix_g[l]), sh_m, sc_m),
            modulate(rms_norm(x_ctx, norm_mix_g[l]), csh_m, csc_m),
            w_in[l], w_out[l], q_norm_g[l], k_norm_g[l], conv_w[l], w0[l], w_up[l], a0[l], a_up[l], g_up[l],
            k_k[l], k_a[l], r_k[l], ln_x_g[l], ln_x_b[l], cos, sin, last)
        x_lat = x_lat + g_m * y_lat
        h_lat = modulate(rms_norm(x_lat, norm_ffn_g[l]), sh_f, sc_f).reshape(B * T, D)
        moe_w = (router_w[l], router_b[l], e_w1[l], e_b1[l], e_w2[l], e_b2[l])
        if last:
            x_lat = x_lat + g_f * moe_ffn(h_lat, *moe_w).reshape(B, T, D)
        else:
            x_ctx = x_ctx + cg_m * y_ctx
            h_ctx = modulate(rms_norm(x_ctx, norm_ffn_g[l]), csh_f, csc_f).reshape(B * C, D)
            f = moe_ffn(jnp.concatenate([h_lat, h_ctx], axis=0), *moe_w)
            x_lat = x_lat + g_f * f[:B * T].reshape(B, T, D)
            x_ctx = x_ctx + cg_f * f[B * T:].reshape(B, C, D)
    return rms_norm(x_lat, norm_final_g)
```

```python
import numpy as np
from contextlib import ExitStack
import concourse.bass as bass
import concourse.mybir as mybir
from concourse.bass_utils import run_bass_kernel_spmd

F32 = mybir.dt.float32
BF16 = mybir.dt.bfloat16
AF = mybir.ActivationFunctionType
ALU = mybir.AluOpType
AX = mybir.AxisListType

D = 1024
CTX = 256
NDS = 8
EPS = 1e-6


class Buf:
    __slots__ = ("t", "w", "r")

    def __init__(self, t):
        self.t = t
        self.w = None
        self.r = {}

    def __getitem__(self, k):
        return self.t[k]


class KB:
    def __init__(self):
        self.nc = bass.Bass("TRN2", target_bir_lowering=False)
        self.es = ExitStack()
        nc = self.nc
        self.eng = {'pe': nc.tensor, 'act': nc.scalar, 'dve': nc.vector, 'pool': nc.gpsimd, 'sp': nc.sync}
        self.sem = {k: self.es.enter_context(nc.semaphore("s_" + k)) for k in self.eng}
        self.cnt = {k: 0 for k in self.eng}
        self.waited = {k: {} for k in self.eng}
        self.dsem = {}
        for q in ('sp', 'pool', 'act'):
            self.dsem[q] = [[self.es.enter_context(nc.semaphore(f"d_{q}{i}")), 0] for i in range(NDS)]
        self.dptr = {q: 0 for q in self.dsem}
        self.nbuf = 0
        self.override = {}
        self.sfx = {}
        self.registry = {}
        self.banks = [Buf(self.es.enter_context(nc.psum_tensor(f"bank{i}", [128, 512], F32))) for i in range(8)]

    def sb(self, shape, dt, name=None):
        self.nbuf += 1
        return Buf(self.es.enter_context(self.nc.sbuf_tensor(name or f"sb{self.nbuf}", list(shape), dt)))

    def ring(self, n, shape, dt, name=None):
        return [self.sb(shape, dt, None if name is None else f"{name}{i}") for i in range(n)]

    def dram(self, name, shape, dt, kind="Internal"):
        if name in self.override:
            return self.override[name]
        full = name + self.sfx.get(name, "")
        if full not in self.registry:
            self.registry[full] = self.nc.dram_tensor(full, list(shape), dt, kind=kind).ap()
        return self.registry[full]

    limit = None
    nops = 0

    def op(self, e, fn, reads=(), writes=(), dma=False):
        self.nops += 1
        if self.limit is not None and self.nops > self.limit:
            return None
        deps = {}

        def add(tok):
            if tok is None:
                return
            key, sem, val = tok
            if key not in deps or deps[key][1] < val:
                deps[key] = (sem, val)

        for b in reads:
            add(b.w)
        for b in writes:
            add(b.w)
            for tok in b.r.values():
                add(tok)
        eng = self.eng[e]
        wt = self.waited[e]
        for key, (sem, val) in deps.items():
            if key == e and e == 'pe':
                continue
            if wt.get(key, 0) >= val:
                continue
            eng.wait_ge(sem, val)
            wt[key] = val
        if dma:
            ring = self.dsem[e]
            i = self.dptr[e]
            key = f"d_{e}{i}"
            if ring[i][1] and wt.get(key, 0) < ring[i][1]:
                eng.wait_ge(ring[i][0], ring[i][1])
                wt[key] = ring[i][1]
        inst = fn(eng)
        if dma:
            self.dptr[e] = (i + 1) % len(ring)
            ring[i][1] += 16
            inst.then_inc(ring[i][0], 16)
            tok = (f"d_{e}{i}", ring[i][0], ring[i][1])
        else:
            self.cnt[e] += 1
            inst.then_inc(self.sem[e], 1)
            tok = (e, self.sem[e], self.cnt[e])
        for b in reads:
            b.r[tok[0]] = tok
        for b in writes:
            b.w = tok
            b.r = {}
        return tok

    def barrier(self):
        for e, eng in self.eng.items():
            wt = self.waited[e]
            for k in self.eng:
                if k != e and self.cnt[k] and wt.get(k, 0) < self.cnt[k]:
                    eng.wait_ge(self.sem[k], self.cnt[k])
                    wt[k] = self.cnt[k]
            for q, ring in self.dsem.items():
                for i, (sem, val) in enumerate(ring):
                    key = f"d_{q}{i}"
                    if val and wt.get(key, 0) < val:
                        eng.wait_ge(sem, val)
                        wt[key] = val

    def push(self):
        self._saved = getattr(self, '_saved', [])
        self._saved.append(self.es)
        self.es = ExitStack()

    def pop(self):
        self.barrier()
        self.es.close()
        self.es = self._saved.pop()

    def finish(self):
        sp = self.eng['sp']
        for q, ring in self.dsem.items():
            for sem, val in ring:
                if val:
                    sp.wait_ge(sem, val)
        for k in self.eng:
            if self.cnt[k]:
                sp.wait_ge(self.sem[k], self.cnt[k])
        self.es.close()


C_ID = 0
C_SEL65 = 128
C_SEL2 = 192
C_W = 448


def make_consts():
    c = np.zeros((128, C_W), np.float32)
    c[:, C_ID:C_ID + 128] = np.eye(128, dtype=np.float32)
    c[64, C_SEL65:C_SEL65 + 64] = 1.0
    c[0, C_SEL2:C_SEL2 + 128] = 1.0
    c[1, C_SEL2 + 128:C_SEL2 + 256] = 1.0
    return c


def rope_tables(T):
    row = np.repeat(np.arange(T // 64), 64)
    col = np.tile(np.arange(64), T // 64)
    pos = np.stack([row, col], axis=-1).astype(np.float32)
    freqs = (np.float32(10000.0) ** (-np.arange(16, dtype=np.float32) / np.float32(16))).astype(np.float32)
    ang = (pos[:, :, None] * freqs).astype(np.float32)
    return np.cos(ang).reshape(T, 32).astype(np.float32), np.sin(ang).reshape(T, 32).astype(np.float32)


def phase_mod(kb, cst, identF, cc_d, wmod_d, bmod2_d, g2a_d, g2b_d, want_bc):
    nc = kb.nc
    bA, bB = kb.banks[0], kb.banks[1]
    fm = kb.sb([128, 4, 8, 2], F32)
    bc = {}
    if want_bc:
        for name in ('gm', 'gf'):
            for r in range(2):
                bc[(name, r)] = kb.sb([128, 1024], F32)
    kb.push()
    cc16 = kb.sb([16, 128], F32)
    kb.op('sp', lambda e: e.dma_start(out=cc16[:], in_=cc_d.rearrange("r (k p) -> (r k) p", p=128)), writes=[cc16], dma=True)
    bmod2 = kb.sb([2, 6144], F32)
    kb.op('sp', lambda e: e.dma_start(out=bmod2[:], in_=bmod2_d), writes=[bmod2], dma=True)
    g2a = kb.sb([2, 1024], F32)
    g2b = kb.sb([2, 1024], F32)
    kb.op('sp', lambda e: e.dma_start(out=g2a[:], in_=g2a_d), writes=[g2a], dma=True)
    kb.op('sp', lambda e: e.dma_start(out=g2b[:], in_=g2b_d), writes=[g2b], dma=True)
    kb.op('pe', lambda e: e.transpose(out=bA[:, 0:16], in_=cc16[:], identity=identF[0:16, 0:16]), reads=[cc16, identF], writes=[bA])
    siluT = kb.sb([128, 16], F32)
    kb.op('act', lambda e: e.activation(out=siluT[:], in_=bA[:, 0:16], func=AF.Silu), reads=[bA], writes=[siluT])
    sview = siluT[:].rearrange("p (r k) -> p k r", k=8)
    wm = kb.ring(2, [128, 8, 512], F32)
    mod_row = kb.sb([2, 6144], F32)
    wsrc = wmod_d.rearrange("(k p) c -> p k c", p=128)
    for cch in range(12):
        w = wm[cch % 2]
        c0 = cch * 512
        kb.op('sp', lambda e: e.dma_start(out=w[:], in_=wsrc[:, :, c0:c0 + 512]), writes=[w], dma=True)
        bank = (bA, bB)[cch % 2]
        for k in range(8):
            kb.op('pe', lambda e: e.matmul(bank[0:2, 0:512], lhsT=sview[:, k, :], rhs=w[:, k, :], start=(k == 0), stop=(k == 7)),
                  reads=[siluT, w], writes=[bank])
        kb.op('dve', lambda e: e.tensor_tensor(out=mod_row[:, c0:c0 + 512], in0=bank[0:2, 0:512], in1=bmod2[:, c0:c0 + 512], op=ALU.add),
              reads=[bank, bmod2], writes=[mod_row])
    Am = kb.sb([2, 1024], F32)
    Af = kb.sb([2, 1024], F32)
    kb.op('dve', lambda e: e.scalar_tensor_tensor(out=Am[:], in0=mod_row[:, 1024:2048], scalar=1.0, in1=g2a[:], op0=ALU.add, op1=ALU.mult),
          reads=[mod_row, g2a], writes=[Am])
    kb.op('dve', lambda e: e.scalar_tensor_tensor(out=Af[:], in0=mod_row[:, 4096:5120], scalar=1.0, in1=g2b[:], op0=ALU.add, op1=ALU.mult),
          reads=[mod_row, g2b], writes=[Af])
    srcs = [(Am, 0), (mod_row, 0), (Af, 0), (mod_row, 3072)]
    for qi, (src, off) in enumerate(srcs):
        for k in range(8):
            col = (qi * 8 + k) * 2
            kb.op('pe', lambda e: e.transpose(out=bA[:, col:col + 2], in_=src[0:2, off + k * 128: off + (k + 1) * 128], identity=identF[0:2, 0:2]),
                  reads=[src, identF], writes=[bA])
    kb.op('dve', lambda e: e.tensor_copy(out=fm[:].rearrange("p a k r -> p (a k r)"), in_=bA[:, 0:64]), reads=[bA], writes=[fm])
    if want_bc:
        sel2 = kb.sb([2, 256], F32)
        kb.op('sp', lambda e: e.dma_start(out=sel2[:], in_=cst[0:2, C_SEL2:C_SEL2 + 256]), writes=[sel2], dma=True)
        for name, off in (('gm', 2048), ('gf', 5120)):
            for r in range(2):
                t = bc[(name, r)]
                for hf in range(2):
                    bank = (bA, bB)[hf]
                    kb.op('pe', lambda e: e.matmul(bank[:, 0:512], lhsT=sel2[:, r * 128:(r + 1) * 128], rhs=mod_row[:, off + hf * 512: off + (hf + 1) * 512], start=True, stop=True),
                          reads=[sel2, mod_row], writes=[bank])
                    kb.op('dve', lambda e: e.tensor_copy(out=t[:, hf * 512:(hf + 1) * 512], in_=bank[:, 0:512]), reads=[bank], writes=[t])
    kb.pop()
    return None, fm, bc


def build_mix(T, with_ctx_q, debug=False, stop_after=None, kb=None, q_half=False):
    own = kb is None
    if own:
        kb = KB()
    nc = kb.nc
    kb.push()
    NTOK = CTX + T
    NT = NTOK // 128
    xin = kb.dram("xin", [NTOK, D], F32, "ExternalInput")
    cc_d = kb.dram("cc", [2, D], F32, "ExternalInput")
    wmod_d = kb.dram("w_mod", [D, 6 * D], F32, "ExternalInput")
    bmod2_d = kb.dram("b_mod2", [2, 6 * D], F32, "ExternalInput")
    g2a_d = kb.dram("gmix2", [2, D], F32, "ExternalInput")
    g2b_d = kb.dram("gffn2", [2, D], F32, "ExternalInput")
    win_d = kb.dram("w_in_c", [D, 1536], F32, "ExternalInput")
    gains_d = kb.dram("gains320", [128, 320], F32, "ExternalInput")
    cos_d = kb.dram("rope_cos", [T, 32], F32, "ExternalInput")
    sin_d = kb.dram("rope_sin", [T, 32], F32, "ExternalInput")
    cst = kb.dram("consts", [128, C_W], F32, "ExternalInput")
    oat_d = kb.dram("oat", [4, 64, T], BF16, "ExternalOutput")
    oatc_d = kb.dram("oatc", [4, 64, CTX], BF16, "ExternalOutput")
    rwp_d = kb.dram("rwproj", [9, 128, NTOK], F32, "ExternalOutput" if debug else "Internal")

    identF = kb.sb([128, 128], F32)
    kb.op('sp', lambda e: e.dma_start(out=identF[:], in_=cst[:, C_ID:C_ID + 128]), writes=[identF], dma=True)
    identB = kb.sb([128, 128], BF16)
    kb.op('dve', lambda e: e.tensor_copy(out=identB[:], in_=identF[:]), reads=[identF], writes=[identB])
    sel65 = kb.sb([65, 64], F32)
    kb.op('sp', lambda e: e.dma_start(out=sel65[:], in_=cst[0:65, C_SEL65:C_SEL65 + 64]), writes=[sel65], dma=True)

    mod_row, fm, _ = phase_mod(kb, cst, identF, cc_d, wmod_d, bmod2_d, g2a_d, g2b_d, False)

    kb.push()
    Wc = kb.sb([128, 8, 1536], BF16)
    wsrc = win_d.rearrange("(k p) c -> p k c", p=128)
    for k in range(8):
        kb.op('pool', lambda e: e.dma_start(out=Wc[:, k, :], in_=wsrc[:, k, :]), writes=[Wc], dma=True)
    gains = kb.sb([128, 320], F32)
    kb.op('sp', lambda e: e.dma_start(out=gains[:], in_=gains_d), writes=[gains], dma=True)

    QT = kb.sb([64, 4, T], BF16)
    QTc = kb.sb([64, 4, CTX], BF16)
    KT = kb.sb([64, NTOK], BF16)
    V1 = kb.sb([128, NT, 65], BF16)
    kb.op('pool', lambda e: e.memset(V1[:, :, 64:65], 1.0), writes=[V1])

    x_ring = kb.ring(3, [128, D], F32)
    junk = kb.sb([128, D], BF16)
    xn_ring = kb.ring(2, [128, D], BF16)
    st_ring = kb.ring(2, [128, 8], F32)
    nT_ring = kb.ring(2, [128, 8, 512], BF16)
    qkv_ring = kb.ring(2, [128, 384], F32)
    tmpa = kb.ring(2, [128, 320], F32)
    tmpb = kb.ring(2, [128, 320], F32)
    qr_ring = kb.ring(2, [128, 320], BF16)
    cs_ring = kb.ring(2, [128, 64], F32)
    fmsb = kb.ring(3, [128, 512], F32)
    B = kb.banks
    b_tp, b_tm, b_q, b_fm = B[2], B[3], B[4], (B[5], B[6])

    groups = [(0, 2, True)] + [(2 + 4 * g, 4, False) for g in range(T // 512)]
    fmcount = 0
    for gi, (t0, nt, isctx) in enumerate(groups):
        r = 1 if isctx else 0
        nTb = nT_ring[gi % 2]
        ntok = nt * 128
        for i in range(nt):
            ti = t0 + i
            xt = x_ring[ti % 3]
            xn = xn_ring[ti % 2]
            st = st_ring[ti % 2]
            kb.op('sp', lambda e: e.dma_start(out=xt[:], in_=xin[ti * 128:(ti + 1) * 128, :]), writes=[xt], dma=True)
            kb.op('act', lambda e: e.activation(out=junk[:], in_=xt[:], func=AF.Square, accum_out=st[:, 0:1]), reads=[xt], writes=[junk, st])
            kb.op('dve', lambda e: e.tensor_scalar(out=st[:, 1:2], in0=st[:, 0:1], scalar1=1.0 / D, scalar2=EPS, op0=ALU.mult, op1=ALU.add), reads=[st], writes=[st])
            kb.op('act', lambda e: e.activation(out=st[:, 1:2], in_=st[:, 1:2], func=AF.Sqrt), reads=[st], writes=[st])
            kb.op('dve', lambda e: e.reciprocal(out=st[:, 2:3], in_=st[:, 1:2]), reads=[st], writes=[st])
            kb.op('act', lambda e: e.activation(out=xn[:], in_=xt[:], func=AF.Copy, scale=st[:, 2:3]), reads=[xt, st], writes=[xn])
            tpb = b_tp[:].bitcast(BF16)
            for k in range(8):
                kb.op('pe', lambda e: e.transpose(out=tpb[:, k * 128:(k + 1) * 128], in_=xn[:, k * 128:(k + 1) * 128], identity=identB[:]),
                      reads=[xn, identB], writes=[b_tp])
            for k in range(8):
                kb.op('act', lambda e: e.activation(out=nTb[:, k, i * 128:(i + 1) * 128], in_=tpb[:, k * 128:(k + 1) * 128], func=AF.Identity,
                                                    scale=fm[:, 0, k, r:r + 1], bias=fm[:, 1, k, r:r + 1]), reads=[b_tp, fm], writes=[nTb])
        for i in range(nt):
            ti = t0 + i
            for k in range(8):
                kb.op('pe', lambda e: e.matmul(b_tm[:, 0:384], lhsT=nTb[:, k, i * 128:(i + 1) * 128], rhs=Wc[:, k, 0:384], start=(k == 0), stop=(k == 7)),
                      reads=[nTb, Wc], writes=[b_tm])
            qkv = qkv_ring[ti % 2]
            ta, tb2, qr, st = tmpa[ti % 2], tmpb[ti % 2], qr_ring[ti % 2], st_ring[ti % 2]
            kb.op('act', lambda e: e.copy(out=qkv[:], in_=b_tm[:, 0:384]), reads=[b_tm], writes=[qkv])
            kb.op('pool', lambda e: e.tensor_copy(out=V1[:, ti, 0:64], in_=qkv[:, 320:384]), reads=[qkv], writes=[V1])
            kb.op('dve', lambda e: e.tensor_tensor(out=ta[:], in0=qkv[:, 0:320], in1=qkv[:, 0:320], op=ALU.mult), reads=[qkv], writes=[ta])
            kb.op('dve', lambda e: e.tensor_reduce(out=st[:, 3:8], in_=ta[:].rearrange("p (h d) -> p h d", d=64), axis=AX.X, op=ALU.add), reads=[ta], writes=[st])
            kb.op('dve', lambda e: e.tensor_scalar(out=st[:, 3:8], in0=st[:, 3:8], scalar1=1.0 / 64, scalar2=EPS, op0=ALU.mult, op1=ALU.add), reads=[st], writes=[st])
            kb.op('act', lambda e: e.activation(out=st[:, 3:8], in_=st[:, 3:8], func=AF.Sqrt), reads=[st], writes=[st])
            kb.op('dve', lambda e: e.reciprocal(out=st[:, 3:8], in_=st[:, 3:8]), reads=[st], writes=[st])
            kb.op('dve', lambda e: e.tensor_tensor(out=ta[:].rearrange("p (h d) -> p h d", d=64), in0=qkv[:, 0:320].rearrange("p (h d) -> p h d", d=64),
                                                   in1=st[:, 3:8].unsqueeze(2).broadcast_to([128, 5, 64]), op=ALU.mult), reads=[qkv, st], writes=[ta])
            if isctx:
                kb.op('dve', lambda e: e.tensor_tensor(out=qr[:], in0=ta[:], in1=gains[:], op=ALU.mult), reads=[ta, gains], writes=[qr])
            else:
                kb.op('dve', lambda e: e.tensor_tensor(out=tb2[:], in0=ta[:], in1=gains[:], op=ALU.mult), reads=[ta, gains], writes=[tb2])
                cs = cs_ring[ti % 2]
                tl = (ti - 2) * 128
                kb.op('sp', lambda e: e.dma_start(out=cs[:, 0:32], in_=cos_d[tl:tl + 128, :]), writes=[cs], dma=True)
                kb.op('sp', lambda e: e.dma_start(out=cs[:, 32:64], in_=sin_d[tl:tl + 128, :]), writes=[cs], dma=True)
                xv = tb2[:].rearrange("p (h a w q) -> p h a w q", h=5, a=2, w=2, q=16)
                ov = qr[:].rearrange("p (h a w q) -> p h a w q", h=5, a=2, w=2, q=16)
                tv = ta[:].rearrange("p (h a w q) -> p h a w q", h=5, a=2, w=2, q=16)
                cv = cs[:, 0:32].rearrange("p (a q) -> p a q", a=2).unsqueeze(1).broadcast_to([128, 5, 2, 16])
                sv = cs[:, 32:64].rearrange("p (a q) -> p a q", a=2).unsqueeze(1).broadcast_to([128, 5, 2, 16])
                x1, x2 = xv[:, :, :, 0, :], xv[:, :, :, 1, :]
                kb.op('dve', lambda e: e.tensor_tensor(out=tv[:, :, :, 0, :], in0=x2, in1=sv, op=ALU.mult), reads=[tb2, cs], writes=[ta])
                kb.op('dve', lambda e: e.tensor_tensor(out=tv[:, :, :, 1, :], in0=x1, in1=sv, op=ALU.mult), reads=[tb2, cs], writes=[ta])
                kb.op('dve', lambda e: e.tensor_tensor(out=x1, in0=x1, in1=cv, op=ALU.mult), reads=[tb2, cs, ta], writes=[tb2])
                kb.op('dve', lambda e: e.tensor_tensor(out=x2, in0=x2, in1=cv, op=ALU.mult), reads=[tb2, cs], writes=[tb2])
                kb.op('dve', lambda e: e.tensor_tensor(out=ov[:, :, :, 0, :], in0=x1, in1=tv[:, :, :, 0, :], op=ALU.subtract), reads=[tb2, ta], writes=[qr])
                kb.op('dve', lambda e: e.tensor_tensor(out=ov[:, :, :, 1, :], in0=x2, in1=tv[:, :, :, 1, :], op=ALU.add), reads=[tb2, ta], writes=[qr])
            qb = b_q[:].bitcast(BF16)
            for h in range(5):
                kb.op('pe', lambda e: e.transpose(out=qb[0:64, h * 128:(h + 1) * 128], in_=qr[:, h * 64:(h + 1) * 64], identity=identB[:]),
                      reads=[qr, identB], writes=[b_q])
            if isctx:
                if with_ctx_q:
                    kb.op('act', lambda e: e.copy(out=QTc[:, :, ti * 128:(ti + 1) * 128], in_=qb[0:64, 0:512].rearrange("p (h t) -> p h t", h=4)), reads=[b_q], writes=[QTc])
            else:
                tl = (ti - 2) * 128
                kb.op('act', lambda e: e.copy(out=QT[:, :, tl:tl + 128], in_=qb[0:64, 0:512].rearrange("p (h t) -> p h t", h=4)), reads=[b_q], writes=[QT])
            kb.op('act', lambda e: e.copy(out=KT[:, ti * 128:(ti + 1) * 128], in_=qb[0:64, 512:640]), reads=[b_q], writes=[KT])
        for j in range(9):
            bank = b_fm[fmcount % 2]
            sbt = fmsb[fmcount % 3]
            for k in range(8):
                kb.op('pe', lambda e: e.matmul(bank[:, 0:ntok], lhsT=Wc[:, k, 384 + j * 128: 384 + (j + 1) * 128], rhs=nTb[:, k, 0:ntok], start=(k == 0), stop=(k == 7)),
                      reads=[Wc, nTb], writes=[bank])
            if fmcount % 2 == 0:
                kb.op('act', lambda e: e.copy(out=sbt[:, 0:ntok], in_=bank[:, 0:ntok]), reads=[bank], writes=[sbt])
            else:
                kb.op('dve', lambda e: e.tensor_copy(out=sbt[:, 0:ntok], in_=bank[:, 0:ntok]), reads=[bank], writes=[sbt])
            kb.op('pool', lambda e: e.dma_start(out=rwp_d[j, :, t0 * 128: t0 * 128 + ntok], in_=sbt[:, 0:ntok]), reads=[sbt], dma=True)
            fmcount += 1

    if stop_after == 'proj':
        kb.pop()
        kb.pop()
        kb.finish()
        return nc
    bS = (B[0], B[1], B[2])
    bO = (B[3], B[4])
    bB = B[5]
    pT_ring = kb.ring(3, [128, 512], BF16)
    osb_ring = kb.ring(2, [65, 512], F32)
    rl_ring = kb.ring(2, [64, 512], F32)
    ot_ring = kb.ring(2, [64, 512], BF16)
    jobs = []
    if with_ctx_q:
        for h in range(4):
            jobs.append((QTc, h, 0, CTX, 2, oatc_d))
    TQ = T
    if q_half:
        TQ = T // 2
        msel_d = kb.dram("msel", [128, 2], F32, "ExternalInput")
        msel = kb.sb([128, 2], F32)
        kb.op('sp', lambda e: e.dma_start(out=msel[:], in_=msel_d), writes=[msel], dma=True)
        for h in range(4):
            kb.op('dve', lambda e: e.tensor_scalar(out=QT[:, h, 0:TQ], in0=QT[:, h, 0:TQ], scalar1=msel[0:64, 0:1], scalar2=None, op0=ALU.mult), reads=[QT, msel], writes=[QT])
            kb.op('dve', lambda e: e.scalar_tensor_tensor(out=QT[:, h, 0:TQ], in0=QT[:, h, TQ:T], scalar=msel[0:64, 1:2], in1=QT[:, h, 0:TQ], op0=ALU.mult, op1=ALU.add), reads=[QT, msel], writes=[QT])
    qblk = min(512, TQ)
    for qb_i in range(TQ // qblk):
        for h in range(4):
            jobs.append((QT, h, qb_i * qblk, qblk, NT, oat_d))
    sc = 0
    for ji, (Qsrc, h, q0, nq, nkt, dst) in enumerate(jobs):
        pso = bO[ji % 2]

        def issue_S(kt, sidx):
            bank = bS[sidx % 3]
            kb.op('pe', lambda e: e.matmul(bank[:, 0:nq], lhsT=KT[:, kt * 128:(kt + 1) * 128], rhs=Qsrc[:, h, q0:q0 + nq], start=True, stop=True),
                  reads=[KT, Qsrc], writes=[bank])
        issue_S(0, sc)
        if nkt > 1:
            issue_S(1, sc + 1)
        for kt in range(nkt):
            bank = bS[(sc + kt) % 3]
            pT = pT_ring[(sc + kt) % 3]
            kb.op('act', lambda e: e.activation(out=pT[:, 0:nq], in_=bank[:, 0:nq], func=AF.Exp, scale=0.125), reads=[bank], writes=[pT])
            if kt + 2 < nkt:
                issue_S(kt + 2, sc + kt + 2)
            kb.op('pe', lambda e: e.matmul(pso[0:65, 0:nq], lhsT=V1[:, kt, 0:65], rhs=pT[:, 0:nq], start=(kt == 0), stop=(kt == nkt - 1)),
                  reads=[V1, pT], writes=[pso])
        sc += nkt
        osb, rl, ot = osb_ring[ji % 2], rl_ring[ji % 2], ot_ring[ji % 2]
        kb.op('dve', lambda e: e.tensor_copy(out=osb[:, 0:nq], in_=pso[0:65, 0:nq]), reads=[pso], writes=[osb])
        kb.op('pe', lambda e: e.matmul(bB[0:64, 0:nq], lhsT=sel65[:], rhs=osb[:, 0:nq], start=True, stop=True), reads=[sel65, osb], writes=[bB])
        kb.op('dve', lambda e: e.reciprocal(out=rl[:, 0:nq], in_=bB[0:64, 0:nq]), reads=[bB], writes=[rl])
        kb.op('dve', lambda e: e.tensor_tensor(out=ot[:, 0:nq], in0=osb[0:64, 0:nq], in1=rl[:, 0:nq], op=ALU.mult), reads=[osb, rl], writes=[ot])
        kb.op('pool', lambda e: e.dma_start(out=dst[h, :, q0:q0 + nq], in_=ot[:, 0:nq]), reads=[ot], dma=True)
    kb.pop()
    if stop_after == 'attn':
        kb.pop()
        kb.finish()
        return nc
    phase_rwkv(kb, T, with_ctx_q, identF, rwp_d, debug)
    kb.pop()
    if own:
        kb.finish()
    return nc


def mix_inputs(T, l, b, hh, xl, xc, inp):
    w_in = inp['w_in'][l]
    sl = lambda a, n: w_in[:, a:a + n]
    w_in_c = np.concatenate([
        sl(1536 + hh * 256, 256), sl(hh * 64, 64), sl(128 + hh * 64, 64),
        sl(256 + hh * 256, 256), sl(768 + hh * 256, 256), sl(2048 + hh * 256, 256),
        sl(1280, 128), sl(1408, 128), sl(2560, 128)], axis=1)
    gains = np.concatenate([np.tile(inp['q_norm_g'][l], 4), inp['k_norm_g'][l]])
    cos, sin = rope_tables(T)
    return {
        "xin": np.ascontiguousarray(np.concatenate([xc[b], xl[b]], axis=0)),
        "cc": np.ascontiguousarray(np.stack([inp['c'][b], inp['c_ctx']])),
        "w_mod": np.ascontiguousarray(inp['w_mod'][l]),
        "b_mod2": np.ascontiguousarray(np.tile(inp['b_mod'][l][None], (2, 1))),
        "gmix2": np.ascontiguousarray(np.tile(inp['norm_mix_g'][l][None], (2, 1))),
        "gffn2": np.ascontiguousarray(np.tile(inp['norm_ffn_g'][l][None], (2, 1))),
        "w_in_c": np.ascontiguousarray(w_in_c),
        "gains320": np.ascontiguousarray(np.tile(gains[None], (128, 1))),
        "rope_cos": cos, "rope_sin": sin,
        "consts": make_consts(),
        **rwkv_inputs(l, hh, inp),
    }


RC_BONES = 0
RC_BONES64 = 128
RC_TRI_F = 256
RC_TRI_B = 512
RC_MASK_F = 768
RC_MASK_B = 1280
RC_W = 1792
GN_EPS = 64e-5
LCH = 64


def make_rconsts():
    c = np.zeros((128, RC_W), np.float32)
    bd = np.kron(np.eye(2, dtype=np.float32), np.ones((64, 64), np.float32))
    c[:, RC_BONES:RC_BONES + 128] = bd
    c[:, RC_BONES64:RC_BONES64 + 128] = bd / 64.0
    s = np.arange(64)[:, None]
    t = np.arange(64)[None, :]
    cdec = -np.exp(np.float32(-0.5))
    for off, incl, excl in ((RC_TRI_F, s <= t, s < t), (RC_TRI_B, s >= t, s > t)):
        blk = np.concatenate([incl, excl], axis=1).astype(np.float32) * cdec
        c[0:64, off:off + 128] = blk
        c[64:128, off + 128:off + 256] = blk
    e2 = np.eye(2, dtype=np.float32)
    SU = np.kron(e2, (s < t).astype(np.float32))
    IU = np.kron(e2, (s <= t).astype(np.float32))
    SL = np.kron(e2, (s > t).astype(np.float32))
    IL = np.kron(e2, (s >= t).astype(np.float32))
    c[:, RC_MASK_F:RC_MASK_F + 512] = np.concatenate([SU, IU, SL, SL], axis=1)
    c[:, RC_MASK_B:RC_MASK_B + 512] = np.concatenate([SL, IL, SU, SU], axis=1)
    return c


def phase_rwkv(kb, T, with_ctx, identF, rwp_d, debug):
    NTOK = CTX + T
    kb.push()
    cw_d = kb.dram("rw_cw", [128, 18], F32, "ExternalInput")
    wupA_d = kb.dram("rw_wupA", [2, 65, 256], F32, "ExternalInput")
    aupA_d = kb.dram("rw_aupA", [2, 65, 256], F32, "ExternalInput")
    gup_d = kb.dram("rw_gup", [128, 256], F32, "ExternalInput")
    cv_d = kb.dram("rw_cv", [128, 10], F32, "ExternalInput")
    rc_d = kb.dram("rconsts", [128, RC_W], F32, "ExternalInput")
    orw_d = kb.dram("orw", [4, 64, T], BF16, "ExternalOutput")
    orwc_d = kb.dram("orwc", [4, 64, CTX], BF16, "ExternalOutput")
    odir_d = kb.dram("odir", [2, 2, 128, NTOK], F32, "ExternalOutput" if debug else "Internal")

    def load(shape, src, q='sp'):
        t = kb.sb(shape, F32)
        kb.op(q, lambda e: e.dma_start(out=t[:], in_=src), writes=[t], dma=True)
        return t
    RC = load([128, RC_W], rc_d)
    convw = load([128, 18], cw_d)
    cv = load([128, 10], cv_d)
    gup = load([128, 256], gup_d)
    wupA = kb.sb([65, 2, 256], F32)
    aupA = kb.sb([65, 2, 256], F32)
    for d in range(2):
        kb.op('sp', lambda e: e.dma_start(out=wupA[:, d, :], in_=wupA_d[d]), writes=[wupA], dma=True)
        kb.op('sp', lambda e: e.dma_start(out=aupA[:, d, :], in_=aupA_d[d]), writes=[aupA], dma=True)
    dv = kb.sb([128, 2, 2], F32)
    for p in range(2):
        kb.op('dve', lambda e: e.tensor_scalar(out=dv[:, p, 0:1], in0=cv[:, p * 5 + 1:p * 5 + 2], scalar1=-1.0, scalar2=1.0, op0=ALU.mult, op1=ALU.add), reads=[cv], writes=[dv])
        kb.op('dve', lambda e: e.tensor_scalar(out=dv[:, p, 1:2], in0=cv[:, p * 5 + 1:p * 5 + 2], scalar1=0.5, scalar2=None, op0=ALU.mult), reads=[cv], writes=[dv])
    BONES = RC[:, RC_BONES:RC_BONES + 128]
    BONES64 = RC[:, RC_BONES64:RC_BONES64 + 128]
    maskBD4 = lambda nch: RC[:, RC_BONES:RC_BONES + 128].rearrange("p (h s) -> p h s", h=2).unsqueeze(1).broadcast_to([128, nch, 2, 64])

    B = kb.banks
    hctr = [0]

    def half():
        i = hctr[0] % 8
        hctr[0] += 1
        t = B[i].t
        return B[i], (lambda a, b, t=t: t[:, a:b])

    groups = [(0, CTX, True)] + [(CTX + 512 * g, 512, False) for g in range(T // 512)]

    kb.push()
    raw = kb.ring(3, [128, 514], F32)
    cq = kb.ring(3, [128, 512], F32)
    xwd = kb.sb([65, 512], F32)
    xad = kb.ring(2, [65, 512], F32)
    kb.op('pool', lambda e: e.memset(xwd[64:65, :], 1.0), writes=[xwd])
    for x_ in xad:
        kb.op('pool', lambda e: e.memset(x_[64:65, :], 1.0), writes=[x_])
    sg = kb.sb([128, 4, 128], F32)
    a_sb = kb.ring(2, [128, 512], F32)
    cw_sb = kb.sb([128, 8, 128], F32)
    Es = kb.ring(4, [128, 512], F32)
    dl = kb.sb([128, 512], F32)
    tA = kb.ring(6, [128, 512], F32)
    BDall = kb.ring(2, [128, 8, 6, 128], F32)
    BDV = kb.ring(2, [128, 8, 128], F32)
    E1keep = kb.ring(2, [128, 512], F32)
    NB = 4
    XR = kb.ring(NB, [128, 256], F32)
    YA = kb.ring(NB, [128, 256], F32)
    AK = kb.ring(NB, [128, 256], F32)
    Xp = [kb.ring(NB, [128, 128], F32) for _ in range(2)]
    Yp = [kb.ring(NB, [128, 128], F32) for _ in range(2)]
    Tp = [kb.ring(NB, [128, 128], F32) for _ in range(2)]
    AtT = kb.ring(NB, [128, 128], F32)
    BhT = kb.ring(NB, [128, 128], F32)
    Esb = kb.ring(NB, [128, 256], F32)
    VT = [kb.ring(NB, [128, 128], F32) for _ in range(2)]
    Qc = [kb.ring(NB, [128, 128], F32) for _ in range(2)]
    Mc = [kb.ring(NB, [128, 128], F32) for _ in range(2)]
    Fsb = [kb.ring(NB, [128, 256], F32) for _ in range(2)]
    ST = [kb.ring(2, [128, 128], F32) for _ in range(2)]
    ostage = kb.ring(2, [128, 512], F32)

    def conv(dst, src, col, ntok):
        kb.op('pool', lambda e: e.tensor_scalar(out=dst[:, 0:ntok], in0=src[:, 0:ntok], scalar1=convw[:, col:col + 1], scalar2=None, op0=ALU.mult), reads=[src, convw], writes=[dst])
        for tap in (1, 2):
            kb.op('dve', lambda e: e.scalar_tensor_tensor(out=dst[:, 0:ntok], in0=src[:, tap:tap + ntok], scalar=convw[:, col + tap:col + tap + 1], in1=dst[:, 0:ntok], op0=ALU.mult, op1=ALU.add),
                  reads=[src, convw, dst], writes=[dst])

    def load_raw(q, j, t0, ntok, ring=None):
        r_ = (ring or raw)[q]
        left_zero = (t0 == 0) or (t0 == CTX)
        right_zero = (t0 + ntok == CTX) or (t0 + ntok == NTOK)
        a = 1 if left_zero else 0
        b = ntok + 1 if right_zero else ntok + 2
        if left_zero:
            kb.op('pool', lambda e: e.memset(r_[:, 0:1], 0.0), writes=[r_])
        if right_zero:
            kb.op('pool', lambda e: e.memset(r_[:, ntok + 1:ntok + 2], 0.0), writes=[r_])
        kb.op('sp', lambda e: e.dma_start(out=r_[:, a:b], in_=rwp_d[j, :, t0 - 1 + a: t0 - 1 + b]), writes=[r_], dma=True)
        return r_

    def sig_a(dst, xa_t, d, p, t0, ntok):
        kb.op('sp', lambda e: e.dma_start(out=xa_t[0:64, 0:ntok], in_=rwp_d[7, d * 64:(d + 1) * 64, t0:t0 + ntok]), writes=[xa_t], dma=True)
        hb, hv = half()
        kb.op('pe', lambda e: e.matmul(hv(0, ntok), lhsT=aupA[:, d, p * 128:(p + 1) * 128], rhs=xa_t[:, 0:ntok], start=True, stop=True), reads=[aupA, xa_t], writes=[hb])
        kb.op('act', lambda e: e.activation(out=dst[:, 0:ntok], in_=hv(0, ntok), func=AF.Sigmoid), reads=[hb], writes=[dst])

    def prep(d, grp, p, slot):
        t0, ntok, isctx = grp
        nch = ntok // LCH
        ntile = ntok // 128
        TRI = RC_TRI_F if d == 0 else RC_TRI_B
        Lidx = LCH - 1 if d == 0 else 0
        bd = BDall[slot]
        v3 = lambda t: t[:, 0:ntok].rearrange("p (c s) -> p c s", s=LCH)
        for q, j in enumerate((0 + p, 2 + p, 4 + p)):
            r_ = load_raw(q, j, t0, ntok)
            conv(cq[q], r_, p * 9 + q * 3, ntok)
        k_, v_, r_c = cq
        kb.op('sp', lambda e: e.dma_start(out=xwd[0:64, 0:ntok], in_=rwp_d[6, d * 64:(d + 1) * 64, t0:t0 + ntok]), writes=[xwd], dma=True)
        kb.op('act', lambda e: e.activation(out=xwd[0:64, 0:ntok], in_=xwd[0:64, 0:ntok], func=AF.Tanh), reads=[xwd], writes=[xwd])
        hb, hv = half()
        for i in range(ntile):
            kb.op('pe', lambda e: e.matmul(hv(i * 128, (i + 1) * 128), lhsT=xwd[:, i * 128:(i + 1) * 128], rhs=wupA[:, d, p * 128:(p + 1) * 128], start=True, stop=True),
                  reads=[xwd, wupA], writes=[hb])
        kb.op('act', lambda e: e.activation(out=sg[:, 0:ntile, :], in_=hv(0, ntile * 128).rearrange("p (i c) -> p i c", c=128), func=AF.Sigmoid), reads=[hb], writes=[sg])
        cwb = [half() for _ in range((ntile + 1) // 2)]
        for i in range(ntile):
            hb, hv = cwb[i // 2]
            cc = (i % 2) * 256
            kb.op('pe', lambda e: e.matmul(hv(cc, cc + 256), lhsT=sg[:, i, :], rhs=RC[:, TRI:TRI + 256], start=True, stop=True),
                  reads=[sg, RC], writes=[hb])
        for bi in range((nch + 3) // 4):
            n_ = min(4, nch - bi * 4)
            hb, hv = cwb[bi]
            kb.op('act', lambda e: e.copy(out=cw_sb[:, bi * 4:bi * 4 + n_, :], in_=hv(0, n_ * 128).rearrange("p (c s) -> p c s", s=128)), reads=[hb], writes=[cw_sb])
        E1, E2, E3, EL = Es
        kb.op('dve', lambda e: e.tensor_tensor(out=v3(dl), in0=cw_sb[:, 0:nch, Lidx:Lidx + 1].broadcast_to([128, nch, LCH]), in1=cw_sb[:, 0:nch, 0:LCH], op=ALU.subtract), reads=[cw_sb], writes=[dl])
        kb.op('act', lambda e: e.activation(out=v3(E1), in_=cw_sb[:, 0:nch, 0:LCH], func=AF.Exp), reads=[cw_sb], writes=[E1])
        kb.op('act', lambda e: e.activation(out=v3(E2), in_=cw_sb[:, 0:nch, 0:LCH], func=AF.Exp, scale=-1.0), reads=[cw_sb], writes=[E2])
        kb.op('act', lambda e: e.activation(out=v3(E3), in_=cw_sb[:, 0:nch, LCH:2 * LCH], func=AF.Exp), reads=[cw_sb], writes=[E3])
        kb.op('act', lambda e: e.activation(out=EL[:, 0:ntok], in_=dl[:, 0:ntok], func=AF.Exp), reads=[dl], writes=[EL])
        kb.op('pool', lambda e: e.tensor_copy(out=E1keep[slot][:, 0:ntok], in_=E1[:, 0:ntok]), reads=[E1], writes=[E1keep[slot]])
        a_ = a_sb[0]
        sig_a(a_, xad[0], d, p, t0, ntok)
        kk, t1, kkn, u_, bop, t2 = tA
        N = slice(0, ntok)
        kb.op('dve', lambda e: e.tensor_scalar(out=kk[:, N], in0=k_[:, N], scalar1=cv[:, p * 5:p * 5 + 1], scalar2=None, op0=ALU.mult), reads=[k_, cv], writes=[kk])
        kb.op('pool', lambda e: e.tensor_tensor(out=t1[:, N], in0=kk[:, N], in1=kk[:, N], op=ALU.mult), reads=[kk], writes=[t1])
        hb, hv = half()
        kb.op('pe', lambda e: e.matmul(hv(0, ntok), lhsT=BONES, rhs=t1[:, N], start=True, stop=True), reads=[RC, t1], writes=[hb])
        kb.op('dve', lambda e: e.tensor_scalar(out=t2[:, N], in0=hv(0, ntok), scalar1=1e-24, scalar2=None, op0=ALU.max), reads=[hb], writes=[t2])
        kb.op('act', lambda e: e.activation(out=t2[:, N], in_=t2[:, N], func=AF.Sqrt), reads=[t2], writes=[t2])
        kb.op('dve', lambda e: e.reciprocal(out=t2[:, N], in_=t2[:, N]), reads=[t2], writes=[t2])
        kb.op('pool', lambda e: e.tensor_tensor(out=kkn[:, N], in0=kk[:, N], in1=t2[:, N], op=ALU.mult), reads=[kk, t2], writes=[kkn])
        kb.op('dve', lambda e: e.tensor_scalar(out=u_[:, N], in0=a_[:, N], scalar1=cv[:, p * 5 + 1:p * 5 + 2], scalar2=dv[:, p, 0:1], op0=ALU.mult, op1=ALU.add), reads=[a_, cv, dv], writes=[u_])
        kb.op('pool', lambda e: e.tensor_tensor(out=u_[:, N], in0=k_[:, N], in1=u_[:, N], op=ALU.mult), reads=[k_, u_], writes=[u_])
        kb.op('pool', lambda e: e.tensor_tensor(out=bop[:, N], in0=kkn[:, N], in1=a_[:, N], op=ALU.mult), reads=[kkn, a_], writes=[bop])
        kdir = u_

        def embed(oi, src):
            kb.op('dve', lambda e: e.tensor_tensor(out=bd[:, 0:nch, oi, :].rearrange("p c (h s) -> p c h s", h=2),
                                                   in0=v3(src).unsqueeze(2).broadcast_to([128, nch, 2, LCH]), in1=maskBD4(nch), op=ALU.mult), reads=[src, RC], writes=[bd])
        kb.op('dve', lambda e: e.scalar_tensor_tensor(out=t1[:, N], in0=kkn[:, N], scalar=-1.0, in1=E3[:, N], op0=ALU.mult, op1=ALU.mult), reads=[kkn, E3], writes=[t1])
        embed(0, t1)
        kb.op('pool', lambda e: e.tensor_tensor(out=t2[:, N], in0=r_c[:, N], in1=E1[:, N], op=ALU.mult), reads=[r_c, E1], writes=[t2])
        embed(1, t2)
        kb.op('pool', lambda e: e.tensor_tensor(out=t1[:, N], in0=bop[:, N], in1=E2[:, N], op=ALU.mult), reads=[bop, E2], writes=[t1])
        embed(2, t1)
        kb.op('pool', lambda e: e.tensor_tensor(out=t2[:, N], in0=kdir[:, N], in1=E2[:, N], op=ALU.mult), reads=[kdir, E2], writes=[t2])
        embed(3, t2)
        kb.op('pool', lambda e: e.tensor_tensor(out=t1[:, N], in0=bop[:, N], in1=EL[:, N], op=ALU.mult), reads=[bop, EL], writes=[t1])
        embed(4, t1)
        kb.op('pool', lambda e: e.tensor_tensor(out=t2[:, N], in0=kdir[:, N], in1=EL[:, N], op=ALU.mult), reads=[kdir, EL], writes=[t2])
        embed(5, t2)
        kb.op('dve', lambda e: e.tensor_tensor(out=BDV[slot][:, 0:nch, :].rearrange("p c (h s) -> p c h s", h=2),
                                               in0=v3(v_).unsqueeze(2).broadcast_to([128, nch, 2, LCH]), in1=maskBD4(nch), op=ALU.mult), reads=[v_, RC], writes=[BDV[slot]])

    ident = identF
    evc = [0]

    def evac_copy(dst_ap, src_ap, rd, wr):
        evc[0] += 1
        if evc[0] % 4:
            kb.op('act', lambda e: e.copy(out=dst_ap, in_=src_ap), reads=rd, writes=wr)
        else:
            kb.op('dve', lambda e: e.tensor_copy(out=dst_ap, in_=src_ap), reads=rd, writes=wr)

    def precompute(d, slot, chunks, bset):
        MASK = RC_MASK_F if d == 0 else RC_MASK_B
        bd = BDall[slot]
        nb = len(chunks)
        op_ = lambda ci, oi: bd[:, ci, oi, :]
        for bi, ci in enumerate(chunks):
            hb, hv = half()
            kb.op('pe', lambda e: e.matmul(hv(0, 256), lhsT=op_(ci, 2), rhs=bd[:, ci, 0:2, :].rearrange("p o s -> p (o s)"), start=True, stop=True), reads=[bd], writes=[hb])
            kb.op('dve', lambda e: e.tensor_tensor(out=XR[bi][:], in0=hv(0, 256), in1=RC[:, MASK:MASK + 256], op=ALU.mult), reads=[hb, RC], writes=[XR[bi]])
            hb, hv = half()
            kb.op('pe', lambda e: e.matmul(hv(0, 256), lhsT=op_(ci, 0), rhs=bd[:, ci, 2:4, :].rearrange("p o s -> p (o s)"), start=True, stop=True), reads=[bd], writes=[hb])
            kb.op('dve', lambda e: e.tensor_tensor(out=YA[bi][:], in0=hv(0, 256), in1=RC[:, MASK + 256:MASK + 512], op=ALU.mult), reads=[hb, RC], writes=[YA[bi]])
            hb, hv = half()
            kb.op('pe', lambda e: e.matmul(hv(0, 128), lhsT=op_(ci, 3), rhs=op_(ci, 1), start=True, stop=True), reads=[bd], writes=[hb])
            kb.op('dve', lambda e: e.tensor_tensor(out=AK[bi][:, 0:128], in0=hv(0, 128), in1=RC[:, MASK + 128:MASK + 256], op=ALU.mult), reads=[hb, RC], writes=[AK[bi]])
            kb.op('pool', lambda e: e.tensor_tensor(out=Tp[0][bi][:], in0=YA[bi][:, 0:128], in1=ident[:], op=ALU.add), reads=[YA[bi], ident], writes=[Tp[0][bi]])
        for bi, ci in enumerate(chunks):
            for src_ap, rd, dst, dcol in ((op_(ci, 0), bd, AtT[bi], 0), (op_(ci, 4), bd, BhT[bi], 0), (op_(ci, 5), bd, AK[bi], 128), (BDV[slot][:, ci, :], BDV[slot], VT[bset][bi], 0)):
                hb, hv = half()
                kb.op('pe', lambda e: e.transpose(out=hv(0, 128), in_=src_ap, identity=ident[:]), reads=[rd, ident], writes=[hb])
                evac_copy(dst[:, dcol:dcol + 128], hv(0, 128), [hb], [dst])
        Xc = [XR[bi][:, 0:128] for bi in range(nb)]
        Xb = [XR[bi] for bi in range(nb)]
        Yc = [YA[bi][:, 0:128] for bi in range(nb)]
        Yb = [YA[bi] for bi in range(nb)]
        tcur = 0
        for lvl in range(1, 6):
            pp = lvl % 2
            nX, nXb, nY, nYb = [], [], [], []
            for bi in range(nb):
                hb, hv = half()
                kb.op('pe', lambda e: e.matmul(hv(0, 128), lhsT=Yc[bi], rhs=Xc[bi], start=True, stop=True), reads=[Yb[bi], Xb[bi]], writes=[hb])
                evac_copy(Xp[pp][bi][:], hv(0, 128), [hb], [Xp[pp][bi]])
                nX.append(Xp[pp][bi][:]); nXb.append(Xp[pp][bi])
                if lvl < 5:
                    hb, hv = half()
                    kb.op('pe', lambda e: e.matmul(hv(0, 128), lhsT=Xc[bi], rhs=Yc[bi], start=True, stop=True), reads=[Yb[bi], Xb[bi]], writes=[hb])
                    evac_copy(Yp[pp][bi][:], hv(0, 128), [hb], [Yp[pp][bi]])
                    nY.append(Yp[pp][bi][:]); nYb.append(Yp[pp][bi])
            for bi in range(nb):
                hb, hv = half()
                told, tnew = Tp[tcur][bi], Tp[1 - tcur][bi]
                kb.op('pe', lambda e: e.matmul(hv(0, 128), lhsT=nX[bi], rhs=told[:], start=True, stop=False), reads=[nXb[bi], told], writes=[hb])
                kb.op('pe', lambda e: e.matmul(hv(0, 128), lhsT=ident[:], rhs=told[:], start=False, stop=True), reads=[ident, told], writes=[hb])
                evac_copy(tnew[:], hv(0, 128), [hb], [tnew])
            tcur = 1 - tcur
            Xc, Xb, Yc, Yb = nX, nXb, nY, nYb
        for bi, ci in enumerate(chunks):
            Tt = Tp[tcur][bi]
            hb, hv = half()
            kb.op('pe', lambda e: e.matmul(hv(0, 128), lhsT=Tt[:], rhs=XR[bi][:, 128:256], start=True, stop=True), reads=[Tt, XR[bi]], writes=[hb])
            kb.op('pe', lambda e: e.matmul(hv(128, 256), lhsT=Tt[:], rhs=BhT[bi][:], start=True, stop=True), reads=[Tt, BhT[bi]], writes=[hb])
            evac_copy(Esb[bi][:], hv(0, 256), [hb], [Esb[bi]])
        for bi, ci in enumerate(chunks):
            hb, hv = half()
            kb.op('pe', lambda e: e.matmul(hv(0, 128), lhsT=AtT[bi][:], rhs=Esb[bi][:, 0:128], start=True, stop=False), reads=[AtT[bi], Esb[bi]], writes=[hb])
            kb.op('pe', lambda e: e.matmul(hv(0, 128), lhsT=ident[:], rhs=op_(ci, 1), start=False, stop=True), reads=[ident, bd], writes=[hb])
            kb.op('pe', lambda e: e.matmul(hv(128, 256), lhsT=AtT[bi][:], rhs=Esb[bi][:, 128:256], start=True, stop=True), reads=[AtT[bi], Esb[bi]], writes=[hb])
            kb.op('dve', lambda e: e.tensor_copy(out=Qc[bset][bi][:], in_=hv(0, 128)), reads=[hb], writes=[Qc[bset][bi]])
            wl_col = ci * LCH + (LCH - 1 if d == 0 else 0)
            kb.op('dve', lambda e: e.scalar_tensor_tensor(out=Mc[bset][bi][:], in0=ident[:], scalar=E1keep[slot][:, wl_col:wl_col + 1], in1=hv(128, 256), op0=ALU.mult, op1=ALU.add),
                  reads=[hb, ident, E1keep[slot]], writes=[Mc[bset][bi]])
            hb, hv = half()
            kb.op('pe', lambda e: e.matmul(hv(0, 256), lhsT=YA[bi][:, 128:256], rhs=Esb[bi][:], start=True, stop=False), reads=[YA[bi], Esb[bi]], writes=[hb])
            kb.op('pe', lambda e: e.matmul(hv(0, 256), lhsT=ident[:], rhs=AK[bi][:], start=False, stop=True), reads=[ident, AK[bi]], writes=[hb])
            evac_copy(Fsb[bset][bi][:], hv(0, 256), [hb], [Fsb[bset][bi]])

    def seq_step(p, bset, bi, sidx, ost, col):
        Scur, Snext = ST[p][sidx % 2], ST[p][(sidx + 1) % 2]
        hb, hv = half()
        kb.op('pe', lambda e: e.matmul(hv(0, 128), lhsT=VT[bset][bi][:], rhs=Fsb[bset][bi][:, 0:128], start=True, stop=False), reads=[VT[bset][bi], Fsb[bset][bi]], writes=[hb])
        kb.op('pe', lambda e: e.matmul(hv(0, 128), lhsT=Scur[:], rhs=Qc[bset][bi][:], start=False, stop=True), reads=[Scur, Qc[bset][bi]], writes=[hb])
        kb.op('dve', lambda e: e.tensor_reduce(out=ost[:, col:col + LCH], in_=hv(0, 128).rearrange("p (h t) -> p t h", h=2), axis=AX.X, op=ALU.add), reads=[hb], writes=[ost])
        hb, hv = half()
        kb.op('pe', lambda e: e.matmul(hv(0, 128), lhsT=Fsb[bset][bi][:, 128:256], rhs=VT[bset][bi][:], start=True, stop=False), reads=[VT[bset][bi], Fsb[bset][bi]], writes=[hb])
        kb.op('pe', lambda e: e.matmul(hv(0, 128), lhsT=Mc[bset][bi][:], rhs=Scur[:], start=False, stop=True), reads=[Scur, Mc[bset][bi]], writes=[hb])
        kb.op('act', lambda e: e.copy(out=Snext[:], in_=hv(0, 128)), reads=[hb], writes=[Snext])

    bctr = 0
    for d in range(2):
        order = [groups[0]] + (groups[1:] if d == 0 else groups[1:][::-1])
        sidx = [0, 0]
        for p in range(2):
            kb.op('pool', lambda e: e.memset(ST[p][0][:], 0.0), writes=[ST[p][0]])
        for grp in order:
            t0, ntok, isctx = grp
            nch = ntok // LCH
            chs = list(range(nch)) if d == 0 else list(range(nch))[::-1]
            for p in range(2):
                prep(d, grp, p, p)
            for b0 in range(0, nch, NB):
                batch = chs[b0:b0 + NB]
                sets = []
                for p in range(2):
                    bset = bctr % 2
                    bctr += 1
                    precompute(d, p, batch, bset)
                    sets.append(bset)
                for bi, ci in enumerate(batch):
                    for p in range(2):
                        seq_step(p, sets[p], bi, sidx[p], ostage[p], ci * LCH)
                        sidx[p] += 1
            for p in range(2):
                kb.op('pool', lambda e: e.dma_start(out=odir_d[d, p, :, t0:t0 + ntok], in_=ostage[p][:, 0:ntok]), reads=[ostage[p]], dma=True)

    kb.pop()
    kb.push()
    NS = 2
    rawS = [kb.ring(3, [128, 514], F32) for _ in range(NS)]
    cqS = [kb.ring(3, [128, 512], F32) for _ in range(NS)]
    aS = [kb.ring(2, [128, 512], F32) for _ in range(NS)]
    xadS = [kb.ring(2, [65, 512], F32) for _ in range(NS)]
    for xs_ in xadS:
        for x_ in xs_:
            kb.op('pool', lambda e: e.memset(x_[64:65, :], 1.0), writes=[x_])
    tS = [kb.ring(6, [128, 512], F32) for _ in range(NS)]
    obufS = [kb.ring(2, [128, 512], F32) for _ in range(NS)]
    xgS = kb.ring(NS, [128, 512], F32)
    ob16S = kb.ring(NS, [128, 512], BF16)
    it = 0
    for grp in groups:
        t0, ntok, isctx = grp
        if isctx and not with_ctx:
            continue
        N = slice(0, ntok)
        for p in range(2):
            si = it % NS
            it += 1
            cq_, a_s, xad_, obuf, xg_t, o16 = cqS[si], aS[si], xadS[si], obufS[si], xgS[si], ob16S[si]
            for q, j in enumerate((0 + p, 2 + p, 4 + p)):
                r_ = load_raw(q, j, t0, ntok, rawS[si])
                conv(cq_[q], r_, p * 9 + q * 3, ntok)
            k_, v_, r_c = cq_
            sig_a(a_s[0], xad_[0], 0, p, t0, ntok)
            sig_a(a_s[1], xad_[1], 1, p, t0, ntok)
            kk, t1, kkn, u_, bop, t2 = tS[si]
            kb.op('dve', lambda e: e.tensor_tensor(out=u_[:, N], in0=a_s[0][:, N], in1=a_s[1][:, N], op=ALU.add), reads=[a_s[0], a_s[1]], writes=[u_])
            kb.op('dve', lambda e: e.tensor_scalar(out=u_[:, N], in0=u_[:, N], scalar1=dv[:, p, 1:2], scalar2=dv[:, p, 0:1], op0=ALU.mult, op1=ALU.add), reads=[u_, dv], writes=[u_])
            kb.op('pool', lambda e: e.tensor_tensor(out=u_[:, N], in0=k_[:, N], in1=u_[:, N], op=ALU.mult), reads=[k_, u_], writes=[u_])
            kb.op('dve', lambda e: e.scalar_tensor_tensor(out=t1[:, N], in0=r_c[:, N], scalar=cv[:, p * 5 + 2:p * 5 + 3], in1=u_[:, N], op0=ALU.mult, op1=ALU.mult), reads=[r_c, cv, u_], writes=[t1])
            hb0, hv0 = half()
            kb.op('pe', lambda e: e.matmul(hv0(0, ntok), lhsT=BONES, rhs=t1[:, N], start=True, stop=True), reads=[RC, t1], writes=[hb0])
            kb.op('dve', lambda e: e.tensor_tensor(out=bop[:, N], in0=hv0(0, ntok), in1=v_[:, N], op=ALU.mult), reads=[hb0, v_], writes=[bop])
            for dd_ in range(2):
                kb.op('sp', lambda e: e.dma_start(out=obuf[dd_][:, N], in_=odir_d[dd_, p, :, t0:t0 + ntok]), writes=[obuf[dd_]], dma=True)
            kb.op('pool', lambda e: e.tensor_tensor(out=kk[:, N], in0=obuf[0][:, N], in1=obuf[1][:, N], op=ALU.add), reads=[obuf[0], obuf[1]], writes=[kk])
            hb1, hv1 = half()
            kb.op('pe', lambda e: e.matmul(hv1(0, ntok), lhsT=BONES64, rhs=kk[:, N], start=True, stop=True), reads=[RC, kk], writes=[hb1])
            kb.op('dve', lambda e: e.tensor_tensor(out=kkn[:, N], in0=kk[:, N], in1=hv1(0, ntok), op=ALU.subtract), reads=[kk, hb1], writes=[kkn])
            kb.op('pool', lambda e: e.tensor_tensor(out=t1[:, N], in0=kkn[:, N], in1=kkn[:, N], op=ALU.mult), reads=[kkn], writes=[t1])
            hb2, hv2 = half()
            kb.op('pe', lambda e: e.matmul(hv2(0, ntok), lhsT=BONES64, rhs=t1[:, N], start=True, stop=True), reads=[RC, t1], writes=[hb2])
            kb.op('dve', lambda e: e.tensor_scalar(out=t2[:, N], in0=hv2(0, ntok), scalar1=GN_EPS, scalar2=None, op0=ALU.add), reads=[hb2], writes=[t2])
            kb.op('act', lambda e: e.activation(out=t2[:, N], in_=t2[:, N], func=AF.Sqrt), reads=[t2], writes=[t2])
            kb.op('dve', lambda e: e.reciprocal(out=t2[:, N], in_=t2[:, N]), reads=[t2], writes=[t2])
            kb.op('pool', lambda e: e.tensor_tensor(out=kkn[:, N], in0=kkn[:, N], in1=t2[:, N], op=ALU.mult), reads=[kkn, t2], writes=[kkn])
            kb.op('dve', lambda e: e.tensor_scalar(out=kkn[:, N], in0=kkn[:, N], scalar1=cv[:, p * 5 + 3:p * 5 + 4], scalar2=cv[:, p * 5 + 4:p * 5 + 5], op0=ALU.mult, op1=ALU.add), reads=[kkn, cv], writes=[kkn])
            kb.op('dve', lambda e: e.tensor_tensor(out=kkn[:, N], in0=kkn[:, N], in1=bop[:, N], op=ALU.add), reads=[kkn, bop], writes=[kkn])
            kb.op('sp', lambda e: e.dma_start(out=xg_t[:, N], in_=rwp_d[8, :, t0:t0 + ntok]), writes=[xg_t], dma=True)
            kb.op('act', lambda e: e.activation(out=xg_t[:, N], in_=xg_t[:, N], func=AF.Sigmoid), reads=[xg_t], writes=[xg_t])
            hb3, hv3 = half()
            kb.op('pe', lambda e: e.matmul(hv3(0, ntok), lhsT=gup[:, p * 128:(p + 1) * 128], rhs=xg_t[:, N], start=True, stop=True), reads=[gup, xg_t], writes=[hb3])
            kb.op('dve', lambda e: e.tensor_tensor(out=o16[:, N], in0=kkn[:, N], in1=hv3(0, ntok), op=ALU.mult), reads=[kkn, hb3], writes=[o16])
            dst, tl = (orwc_d, t0) if isctx else (orw_d, t0 - CTX)
            for hh_ in range(2):
                kb.op('pool', lambda e: e.dma_start(out=dst[2 * p + hh_, :, tl:tl + ntok], in_=o16[hh_ * 64:(hh_ + 1) * 64, N]), reads=[o16], dma=True)
    kb.pop()
    kb.pop()


def rwkv_inputs(l, hh, inp):
    ch = slice(hh * 256, (hh + 1) * 256)
    cwv = inp['conv_w'][l]
    cwc = np.zeros((128, 18), np.float32)
    for p in range(2):
        for q, base in enumerate((512, 1024, 0)):
            cols = slice(base + hh * 256 + p * 128, base + hh * 256 + (p + 1) * 128)
            cwc[:, p * 9 + q * 3: p * 9 + q * 3 + 3] = cwv[:, cols].T
    wupA = np.concatenate([inp['w_up'][l][:, :, ch], inp['w0'][l][:, None, ch]], axis=1)
    aupA = np.concatenate([inp['a_up'][l][:, :, ch], inp['a0'][l][:, None, ch]], axis=1)
    vecs = [inp['k_k'][l][ch], inp['k_a'][l][ch], inp['r_k'][l].reshape(-1)[ch], inp['ln_x_g'][l][ch], inp['ln_x_b'][l][ch]]
    cvv = np.zeros((128, 10), np.float32)
    for p in range(2):
        for i, v in enumerate(vecs):
            cvv[:, p * 5 + i] = v[p * 128:(p + 1) * 128]
    return {
        "rw_cw": cwc, "rw_wupA": np.ascontiguousarray(wupA), "rw_aupA": np.ascontiguousarray(aupA),
        "rw_gup": np.ascontiguousarray(inp['g_up'][l][:, ch]), "rw_cv": cvv, "rconsts": make_rconsts(),
    }


SW_LIMIT = 7.0
SW_ALPHA = 1.702
GMAX = 9


def ffn_groups(ntile):
    ng = (ntile + GMAX - 1) // GMAX
    base, rem = ntile // ng, ntile % ng
    out, t = [], 0
    for g in range(ng):
        n = base + (1 if g < rem else 0)
        out.append((t, n))
        t += n
    return out


def build_ffn(T, last, debug=False, n_exp=32, kb=None, ntiles=None, ctx_tiles=None, select=None, oT_attn_half=None):
    own = kb is None
    if own:
        kb = KB()
    nc = kb.nc
    kb.push()
    NLT = T // 256
    NT2 = (NLT + (0 if last else 1)) if ntiles is None else ntiles
    if ctx_tiles is None:
        ctx_tiles = set() if last else {NT2 - 1}
    NTL = NT2 * 128
    oT_d = kb.dram("oT", [16, 64, NTL], BF16, "ExternalInput")
    xres_d = kb.dram("xres", [NTL, D], F32, "ExternalInput")
    cc_d = kb.dram("cc", [2, D], F32, "ExternalInput")
    wmod_d = kb.dram("w_mod", [D, 6 * D], F32, "ExternalInput")
    bmod2_d = kb.dram("b_mod2", [2, 6 * D], F32, "ExternalInput")
    g2a_d = kb.dram("gmix2", [2, D], F32, "ExternalInput")
    g2b_d = kb.dram("gffn2", [2, D], F32, "ExternalInput")
    cst = kb.dram("consts", [128, C_W], F32, "ExternalInput")
    wout_d = kb.dram("w_out", [D, D], F32, "ExternalInput")
    rw_d = kb.dram("router_w", [D, 32], F32, "ExternalInput")
    rb_d = kb.dram("router_b128", [128, 32], F32, "ExternalInput")
    w1_d = kb.dram("e_w1", [32, D, 2 * D], F32, "ExternalInput")
    b1_d = kb.dram("e_b1T", [32, 128, 16], F32, "ExternalInput")
    w2_d = kb.dram("e_w2", [32, D, D], F32, "ExternalInput")
    b2_d = kb.dram("e_b2", [32, D], F32, "ExternalInput")
    gfin_d = kb.dram("gfin128", [128, D], F32, "ExternalInput")
    xout_d = kb.dram("xout", [NTL, D], F32, "ExternalOutput")
    xmid_d = kb.dram("xmid", [NTL, D], F32, "ExternalOutput" if debug else "Internal")

    identF = kb.sb([128, 128], F32)
    kb.op('sp', lambda e: e.dma_start(out=identF[:], in_=cst[:, C_ID:C_ID + 128]), writes=[identF], dma=True)
    if select is not None:
        msel_d = kb.dram("msel", [128, 2], F32, "ExternalInput")
        msel = kb.sb([128, 2], F32)
        kb.op('sp', lambda e: e.dma_start(out=msel[:], in_=msel_d), writes=[msel], dma=True)
    seven = kb.sb([128, 1], F32)
    kb.op('pool', lambda e: e.memset(seven[:], SW_LIMIT), writes=[seven])
    _, fm, bc = phase_mod(kb, cst, identF, cc_d, wmod_d, bmod2_d, g2a_d, g2b_d, True)
    B = kb.banks
    Wr = kb.sb([128, 8, 32], F32)
    kb.op('sp', lambda e: e.dma_start(out=Wr[:], in_=rw_d.rearrange("(k p) c -> p k c", p=128)), writes=[Wr], dma=True)
    rb = kb.sb([128, 32], F32)
    kb.op('sp', lambda e: e.dma_start(out=rb[:], in_=rb_d), writes=[rb], dma=True)
    b2sb = kb.sb([32, D], F32)
    kb.op('sp', lambda e: e.dma_start(out=b2sb[:], in_=b2_d), writes=[b2sb], dma=True)
    b1sb = kb.sb([128, 32, 16], F32)
    kb.op('sp', lambda e: e.dma_start(out=b1sb[:], in_=b1_d.rearrange("e p j -> p e j")), writes=[b1sb], dma=True)
    hT = kb.sb([128, 8, GMAX * 128], BF16)
    G = kb.sb([128, GMAX, 32], F32)
    GT = kb.sb([32, GMAX, 128], F32)
    w1src = w1_d.rearrange("e (k p) c -> e p k c", p=128)
    w2src = w2_d.rearrange("e (k p) c -> e p k c", p=128)

    for (g0, gn) in ffn_groups(NT2):
        gtok = gn * 128
        kb.push()
        Wo = kb.sb([64, 16, D], BF16)
        wosrc = wout_d.rearrange("(h r) c -> r h c", r=64)
        for h4 in range(4):
            kb.op('pool', lambda e: e.dma_start(out=Wo[:, h4 * 4:(h4 + 1) * 4, :], in_=wosrc[:, h4 * 4:(h4 + 1) * 4, :]), writes=[Wo], dma=True)
        oT_ring = kb.ring(2, [64, 16, 128], BF16)
        x_ring = kb.ring(2, [128, D], F32)
        if select is not None:
            oTb_ring = kb.ring(2, [64, 16, 128], BF16)
            xb_ring = kb.ring(2, [128, D], F32)
        x1_ring = kb.ring(2, [128, D], F32)
        xn_ring = kb.ring(2, [128, D], F32)
        junk = kb.sb([128, D], BF16)
        st_ring = kb.ring(2, [128, 24], F32)
        h32_ring = kb.ring(2, [128, 8, 128], F32)
        lg_ring = kb.ring(2, [128, 3, 32], F32)
        oTsrc = oT_d.rearrange("h r t -> r h t")
        for i in range(gn):
            ti = g0 + i
            r = 1 if ti in ctx_tiles else 0
            rows = slice(ti * 128, (ti + 1) * 128)
            ot, xt, x1, xn, st, h32, lg = oT_ring[i % 2], x_ring[i % 2], x1_ring[i % 2], xn_ring[i % 2], st_ring[i % 2], h32_ring[i % 2], lg_ring[i % 2]
            if select is None:
                kb.op('sp', lambda e: e.dma_start(out=ot[:], in_=oTsrc[:, :, ti * 128:(ti + 1) * 128]), writes=[ot], dma=True)
                kb.op('sp', lambda e: e.dma_start(out=xt[:], in_=xres_d[rows, :]), writes=[xt], dma=True)
            else:
                ca, cb = (select[0] + ti) * 128, (select[1] + ti) * 128
                otb, xtb = oTb_ring[i % 2], xb_ring[i % 2]
                if oT_attn_half is None:
                    s0 = 0
                    kb.op('sp', lambda e: e.dma_start(out=ot[:], in_=oTsrc[:, :, ca:ca + 128]), writes=[ot], dma=True)
                    kb.op('sp', lambda e: e.dma_start(out=otb[:], in_=oTsrc[:, :, cb:cb + 128]), writes=[otb], dma=True)
                else:
                    s0 = 8
                    kb.op('sp', lambda e: e.dma_start(out=ot[:, 0:8, :], in_=oT_attn_half.rearrange("h r t -> r h t")[:, :, ti * 128:(ti + 1) * 128]), writes=[ot], dma=True)
                    kb.op('sp', lambda e: e.dma_start(out=ot[:, 8:16, :], in_=oTsrc[:, 8:16, ca:ca + 128]), writes=[ot], dma=True)
                    kb.op('sp', lambda e: e.dma_start(out=otb[:, 8:16, :], in_=oTsrc[:, 8:16, cb:cb + 128]), writes=[otb], dma=True)
                kb.op('sp', lambda e: e.dma_start(out=xt[:], in_=xres_d[ca:ca + 128, :]), writes=[xt], dma=True)
                kb.op('sp', lambda e: e.dma_start(out=xtb[:], in_=xres_d[cb:cb + 128, :]), writes=[xtb], dma=True)
                o2 = lambda t: t[:, s0:16, :].rearrange("p h t -> p (h t)")
                kb.op('dve', lambda e: e.tensor_scalar(out=o2(ot), in0=o2(ot), scalar1=msel[0:64, 0:1], scalar2=None, op0=ALU.mult), reads=[ot, msel], writes=[ot])
                kb.op('dve', lambda e: e.scalar_tensor_tensor(out=o2(ot), in0=o2(otb), scalar=msel[0:64, 1:2], in1=o2(ot), op0=ALU.mult, op1=ALU.add), reads=[ot, otb, msel], writes=[ot])
                kb.op('dve', lambda e: e.tensor_scalar(out=xt[:], in0=xt[:], scalar1=msel[:, 0:1], scalar2=None, op0=ALU.mult), reads=[xt, msel], writes=[xt])
                kb.op('dve', lambda e: e.scalar_tensor_tensor(out=xt[:], in0=xtb[:], scalar=msel[:, 1:2], in1=xt[:], op0=ALU.mult, op1=ALU.add), reads=[xt, xtb, msel], writes=[xt])
            for hf in range(2):
                for h in range(16):
                    kb.op('pe', lambda e: e.matmul(B[hf][:, 0:512], lhsT=ot[:, h, :], rhs=Wo[:, h, hf * 512:(hf + 1) * 512], start=(h == 0), stop=(h == 15)),
                          reads=[ot, Wo], writes=[B[hf]])
                kb.op('dve', lambda e: e.tensor_tensor(out=x1[:, hf * 512:(hf + 1) * 512], in0=B[hf][:, 0:512], in1=bc[('gm', r)][:, hf * 512:(hf + 1) * 512], op=ALU.mult),
                      reads=[B[hf], bc[('gm', r)]], writes=[x1])
            kb.op('pool', lambda e: e.tensor_tensor(out=x1[:], in0=x1[:], in1=xt[:], op=ALU.add), reads=[x1, xt], writes=[x1])
            kb.op('pool', lambda e: e.dma_start(out=xmid_d[rows, :], in_=x1[:]), reads=[x1], dma=True)
            kb.op('act', lambda e: e.activation(out=junk[:], in_=x1[:], func=AF.Square, accum_out=st[:, 0:1]), reads=[x1], writes=[junk, st])
            kb.op('dve', lambda e: e.tensor_scalar(out=st[:, 1:2], in0=st[:, 0:1], scalar1=1.0 / D, scalar2=EPS, op0=ALU.mult, op1=ALU.add), reads=[st], writes=[st])
            kb.op('act', lambda e: e.activation(out=st[:, 1:2], in_=st[:, 1:2], func=AF.Sqrt), reads=[st], writes=[st])
            kb.op('dve', lambda e: e.reciprocal(out=st[:, 2:3], in_=st[:, 1:2]), reads=[st], writes=[st])
            kb.op('pool', lambda e: e.tensor_scalar(out=xn[:], in0=x1[:], scalar1=st[:, 2:3], scalar2=None, op0=ALU.mult), reads=[x1, st], writes=[xn])
            for k in range(8):
                bank = B[2 + k // 4]
                kb.op('pe', lambda e: e.transpose(out=bank[:, (k % 4) * 128:(k % 4 + 1) * 128], in_=xn[:, k * 128:(k + 1) * 128], identity=identF[:]), reads=[xn, identF], writes=[bank])
            for k in range(8):
                bank = B[2 + k // 4]
                kb.op('act', lambda e: e.activation(out=h32[:, k, :], in_=bank[:, (k % 4) * 128:(k % 4 + 1) * 128], func=AF.Identity, scale=fm[:, 2, k, r:r + 1], bias=fm[:, 3, k, r:r + 1]),
                      reads=[bank, fm], writes=[h32])
            kb.op('pool', lambda e: e.tensor_copy(out=hT[:, :, i * 128:(i + 1) * 128], in_=h32[:]), reads=[h32], writes=[hT])
            for k in range(8):
                kb.op('pe', lambda e: e.matmul(B[4][:, 0:32], lhsT=h32[:, k, :], rhs=Wr[:, k, :], start=(k == 0), stop=(k == 7)), reads=[h32, Wr], writes=[B[4]])
            kb.op('dve', lambda e: e.tensor_tensor(out=lg[:, 0, :], in0=B[4][:, 0:32], in1=rb[:], op=ALU.add), reads=[B[4], rb], writes=[lg])
            kb.op('dve', lambda e: e.max(out=st[:, 8:16], in_=lg[:, 0, :]), reads=[lg], writes=[st])
            kb.op('dve', lambda e: e.tensor_scalar(out=st[:, 16:17], in0=st[:, 8:9], scalar1=-1.0, scalar2=None, op0=ALU.mult), reads=[st], writes=[st])
            kb.op('dve', lambda e: e.tensor_scalar(out=lg[:, 1, :], in0=lg[:, 0, :], scalar1=st[:, 11:12], scalar2=None, op0=ALU.is_ge), reads=[lg, st], writes=[lg])
            kb.op('act', lambda e: e.activation(out=lg[:, 2, :], in_=lg[:, 0, :], func=AF.Exp, bias=st[:, 16:17], scale=1.0), reads=[lg, st], writes=[lg])
            kb.op('dve', lambda e: e.tensor_tensor(out=lg[:, 2, :], in0=lg[:, 2, :], in1=lg[:, 1, :], op=ALU.mult), reads=[lg], writes=[lg])
            kb.op('dve', lambda e: e.tensor_reduce(out=st[:, 17:18], in_=lg[:, 2, :], axis=AX.X, op=ALU.add), reads=[lg], writes=[st])
            kb.op('dve', lambda e: e.reciprocal(out=st[:, 18:19], in_=st[:, 17:18]), reads=[st], writes=[st])
            kb.op('dve', lambda e: e.tensor_scalar(out=G[:, i, :], in0=lg[:, 2, :], scalar1=st[:, 18:19], scalar2=None, op0=ALU.mult), reads=[lg, st], writes=[G])
            kb.op('pe', lambda e: e.transpose(out=B[5][0:32, 0:128], in_=G[:, i, :], identity=identF[:]), reads=[G, identF], writes=[B[5]])
            kb.op('act', lambda e: e.copy(out=GT[:, i, :], in_=B[5][0:32, 0:128]), reads=[B[5]], writes=[GT])
        kb.pop()
        kb.push()
        yacc = kb.sb([128, gn, D], F32)
        kb.push()
        W1 = kb.ring(2, [128, 8, 2 * D], BF16)
        W2 = kb.ring(2, [128, 8, D], BF16)
        actT = kb.ring(2, [128, 8, 512], BF16)
        g1r = kb.ring(2, [128, 512], F32)
        sgr = kb.ring(2, [128, 512], BF16)
        l1r = kb.ring(2, [128, 512], F32)
        chunks = []
        c0 = 0
        while c0 < gtok:
            n = min(512, gtok - c0)
            chunks.append((c0, n))
            c0 += n
        jobs = [(e, c) for e in range(n_exp) for c in chunks]

        def load_w(e):
            w1, w2 = W1[e % 2], W2[e % 2]
            for k in range(8):
                kb.op('pool', lambda e_: e_.dma_start(out=w1[:, k, :], in_=w1src[e, :, k, :]), writes=[w1], dma=True)
            for k4 in range(2):
                kb.op('pool', lambda e_: e_.dma_start(out=w2[:, k4 * 4:(k4 + 1) * 4, :], in_=w2src[e, :, k4 * 4:(k4 + 1) * 4, :]), writes=[w2], dma=True)

        ucnt = [0]

        def emit_U(n):
            e, (c0, cn) = jobs[n]
            w1 = W1[e % 2]
            at = actT[n % 2]
            for j in range(8):
                u = ucnt[0]
                ucnt[0] += 1
                bg, bl = B[(u % 2) * 2], B[(u % 2) * 2 + 1]
                g1, sg_, l1 = g1r[u % 2], sgr[u % 2], l1r[u % 2]
                for k in range(8):
                    kb.op('pe', lambda e_: e_.matmul(bg[:, 0:cn], lhsT=w1[:, k, j * 128:(j + 1) * 128], rhs=hT[:, k, c0:c0 + cn], start=(k == 0), stop=(k == 7)), reads=[w1, hT], writes=[bg])
                for k in range(8):
                    kb.op('pe', lambda e_: e_.matmul(bl[:, 0:cn], lhsT=w1[:, k, D + j * 128:D + (j + 1) * 128], rhs=hT[:, k, c0:c0 + cn], start=(k == 0), stop=(k == 7)), reads=[w1, hT], writes=[bl])
                kb.op('dve', lambda e_: e_.tensor_scalar(out=g1[:, 0:cn], in0=bg[:, 0:cn], scalar1=b1sb[:, e, j:j + 1], scalar2=SW_LIMIT, op0=ALU.add, op1=ALU.min), reads=[bg, b1sb], writes=[g1])
                kb.op('act', lambda e_: e_.activation(out=sg_[:, 0:cn], in_=g1[:, 0:cn], func=AF.Sigmoid, scale=SW_ALPHA), reads=[g1], writes=[sg_])
                kb.op('dve', lambda e_: e_.tensor_scalar(out=l1[:, 0:cn], in0=bl[:, 0:cn], scalar1=b1sb[:, e, 8 + j:9 + j], scalar2=SW_LIMIT, op0=ALU.add, op1=ALU.min), reads=[bl, b1sb], writes=[l1])
                kb.op('act', lambda e_: e_.activation(out=l1[:, 0:cn], in_=l1[:, 0:cn], func=AF.Relu, bias=seven[:, 0:1], scale=1.0), reads=[l1, seven], writes=[l1])
                kb.op('dve', lambda e_: e_.tensor_tensor(out=g1[:, 0:cn], in0=g1[:, 0:cn], in1=sg_[:, 0:cn], op=ALU.mult), reads=[g1, sg_], writes=[g1])
                kb.op('dve', lambda e_: e_.scalar_tensor_tensor(out=at[:, j, 0:cn], in0=l1[:, 0:cn], scalar=1.0 - SW_LIMIT, in1=g1[:, 0:cn], op0=ALU.add, op1=ALU.mult), reads=[g1, l1], writes=[at])

        wcnt = [0]

        def emit_W2(n):
            e, (c0, cn) = jobs[n]
            w2 = W2[e % 2]
            at = actT[n % 2]
            for tt in range(cn // 128):
                ti = (c0 // 128) + tt
                for hf in range(2):
                    w = wcnt[0]
                    wcnt[0] += 1
                    bank = B[4 + w % 4]
                    for j in range(8):
                        kb.op('pe', lambda e_: e_.matmul(bank[:, 0:512], lhsT=at[:, j, tt * 128:(tt + 1) * 128], rhs=w2[:, j, hf * 512:(hf + 1) * 512], start=(j == 0), stop=(j == 7)), reads=[at, w2], writes=[bank])
                    ya = yacc[:, ti, hf * 512:(hf + 1) * 512]
                    if e == 0:
                        kb.op('dve', lambda e_: e_.tensor_scalar(out=ya, in0=bank[:, 0:512], scalar1=G[:, ti, e:e + 1], scalar2=None, op0=ALU.mult), reads=[bank, G], writes=[yacc])
                    else:
                        kb.op('dve', lambda e_: e_.scalar_tensor_tensor(out=ya, in0=bank[:, 0:512], scalar=G[:, ti, e:e + 1], in1=ya, op0=ALU.mult, op1=ALU.add), reads=[bank, G, yacc], writes=[yacc])

        load_w(0)
        if n_exp > 1:
            load_w(1)
        emit_U(0)
        for n in range(len(jobs)):
            if n + 1 < len(jobs):
                emit_U(n + 1)
            emit_W2(n)
            e, c = jobs[n]
            if c == chunks[-1] and e + 2 < n_exp:
                load_w(e + 2)
        kb.pop()
        x_ring = kb.ring(2, [128, D], F32)
        gfin = kb.sb([128, D], F32)
        stf = kb.ring(2, [128, 4], F32)
        junk2 = kb.sb([128, D], BF16)
        if last:
            kb.op('sp', lambda e: e.dma_start(out=gfin[:], in_=gfin_d), writes=[gfin], dma=True)
        for i in range(gn):
            ti = g0 + i
            r = 1 if ti in ctx_tiles else 0
            rows = slice(ti * 128, (ti + 1) * 128)
            xt = x_ring[i % 2]
            kb.op('sp', lambda e: e.dma_start(out=xt[:], in_=xmid_d[rows, :]), writes=[xt], dma=True)
            for hf in range(2):
                kb.op('pe', lambda e: e.matmul(B[hf][:, 0:512], lhsT=GT[:, i, :], rhs=b2sb[:, hf * 512:(hf + 1) * 512], start=True, stop=True), reads=[GT, b2sb], writes=[B[hf]])
                ya = yacc[:, i, hf * 512:(hf + 1) * 512]
                kb.op('dve', lambda e: e.tensor_tensor(out=ya, in0=B[hf][:, 0:512], in1=ya, op=ALU.add), reads=[B[hf], yacc], writes=[yacc])
            kb.op('pool', lambda e: e.tensor_tensor(out=yacc[:, i, :], in0=yacc[:, i, :], in1=bc[('gf', r)][:], op=ALU.mult), reads=[yacc, bc[('gf', r)]], writes=[yacc])
            kb.op('pool', lambda e: e.tensor_tensor(out=xt[:], in0=xt[:], in1=yacc[:, i, :], op=ALU.add), reads=[xt, yacc], writes=[xt])
            if last:
                st = stf[i % 2]
                kb.op('act', lambda e: e.activation(out=junk2[:], in_=xt[:], func=AF.Square, accum_out=st[:, 0:1]), reads=[xt], writes=[junk2, st])
                kb.op('dve', lambda e: e.tensor_scalar(out=st[:, 1:2], in0=st[:, 0:1], scalar1=1.0 / D, scalar2=EPS, op0=ALU.mult, op1=ALU.add), reads=[st], writes=[st])
                kb.op('act', lambda e: e.activation(out=st[:, 1:2], in_=st[:, 1:2], func=AF.Sqrt), reads=[st], writes=[st])
                kb.op('dve', lambda e: e.reciprocal(out=st[:, 2:3], in_=st[:, 1:2]), reads=[st], writes=[st])
                kb.op('dve', lambda e: e.scalar_tensor_tensor(out=xt[:], in0=xt[:], scalar=st[:, 2:3], in1=gfin[:], op0=ALU.mult, op1=ALU.mult), reads=[xt, st, gfin], writes=[xt])
            kb.op('pool', lambda e: e.dma_start(out=xout_d[rows, :], in_=xt[:]), reads=[xt], dma=True)
        kb.pop()
    kb.pop()
    if own:
        kb.finish()
    return nc


def ffn_inputs(T, l, b, hh, last, oT_core, xres_core, inp):
    return {
        "oT": oT_core, "xres": xres_core,
        "cc": np.ascontiguousarray(np.stack([inp['c'][b], inp['c_ctx']])),
        "w_mod": np.ascontiguousarray(inp['w_mod'][l]),
        "b_mod2": np.ascontiguousarray(np.tile(inp['b_mod'][l][None], (2, 1))),
        "gmix2": np.ascontiguousarray(np.tile(inp['norm_mix_g'][l][None], (2, 1))),
        "gffn2": np.ascontiguousarray(np.tile(inp['norm_ffn_g'][l][None], (2, 1))),
        "consts": make_consts(),
        "w_out": np.ascontiguousarray(inp['w_out'][l]),
        "router_w": np.ascontiguousarray(inp['router_w'][l]),
        "router_b128": np.ascontiguousarray(np.tile(inp['router_b'][l][None], (128, 1))),
        "e_w1": np.ascontiguousarray(inp['e_w1'][l]),
        "e_b1T": np.ascontiguousarray(inp['e_b1'][l].reshape(32, 16, 128).transpose(0, 2, 1)),
        "e_w2": np.ascontiguousarray(inp['e_w2'][l]),
        "e_b2": np.ascontiguousarray(inp['e_b2'][l]),
        "gfin128": np.ascontiguousarray(np.tile(inp['norm_final_g'][None], (128, 1))),
    }


MIX_HH = ("w_in_c", "rw_cw", "rw_wupA", "rw_aupA", "rw_gup", "rw_cv")
LAYER_NAMES = ("w_mod", "b_mod2", "gmix2", "gffn2", "gains320", "w_out", "router_w", "router_b128", "e_w1", "e_b1T", "e_w2", "e_b2")


def build_fused(T, n_exp=32, debug=False):
    kb = KB()
    nc = kb.nc
    NTOK = CTX + T
    NT = NTOK // 128
    H = T // 2
    xin0 = kb.dram("xin", [NTOK, D], F32, "ExternalInput")
    oall = kb.dram("oall", [16, 64, NTOK], BF16, "Internal")
    xl1 = kb.dram("xl1", [NTOK, D], F32, "ExternalOutput" if debug else "Internal")
    xmid0 = kb.dram("xmid0", [NTOK, D], F32, "Internal")
    xmid1 = kb.dram("xmid1", [H, D], F32, "Internal")
    xfin = kb.dram("xfin", [H, D], F32, "ExternalOutput")
    oallh = kb.dram("oallh", [8, 64, H], BF16, "Internal")
    rwp = kb.dram("rwproj", [9, 128, NTOK], F32, "Internal")
    odir = kb.dram("odir", [2, 2, 128, NTOK], F32, "Internal")
    for l in range(2):
        last = (l == 1)
        xin = xin0 if l == 0 else xl1
        for hh in range(2):
            kb.sfx = {n: f"_l{l}" for n in LAYER_NAMES}
            kb.sfx.update({n: f"_l{l}h{hh}" for n in MIX_HH})
            kb.override = {
                "xin": xin, "rwproj": rwp, "odir": odir,
                "oat": oall[hh * 4:(hh + 1) * 4, :, CTX:NTOK], "oatc": oall[hh * 4:(hh + 1) * 4, :, 0:CTX],
                "orw": oall[8 + hh * 4:8 + (hh + 1) * 4, :, CTX:NTOK], "orwc": oall[8 + hh * 4:8 + (hh + 1) * 4, :, 0:CTX],
            }
            if last:
                kb.override["oat"] = oallh[hh * 4:(hh + 1) * 4]
            build_mix(T, not last, kb=kb, q_half=last)
            kb.barrier()
        kb.sfx = {n: f"_l{l}" for n in LAYER_NAMES}
        if not last:
            kb.override = {"oT": oall, "xres": xin, "xout": xl1, "xmid": xmid0}
            build_ffn(T, False, n_exp=n_exp, kb=kb, ntiles=NT, ctx_tiles={0, 1})
        else:
            kb.override = {"oT": oall, "xres": xin, "xout": xfin, "xmid": xmid1}
            build_ffn(T, True, n_exp=n_exp, kb=kb, ntiles=H // 128, ctx_tiles=set(), select=(2, 2 + H // 128), oT_attn_half=oallh)
        kb.barrier()
    kb.finish()
    return nc


def fused_inputs(T, b, hh, inp):
    xl, xc = inp['x'], inp['ctx']
    cos, sin = rope_tables(T)
    m = {
        "xin": np.ascontiguousarray(np.concatenate([xc[b], xl[b]], axis=0)),
        "cc": np.ascontiguousarray(np.stack([inp['c'][b], inp['c_ctx']])),
        "consts": make_consts(), "rconsts": make_rconsts(), "rope_cos": cos, "rope_sin": sin,
        "gfin128": np.ascontiguousarray(np.tile(inp['norm_final_g'][None], (128, 1))),
        "msel": np.ascontiguousarray(np.tile(np.eye(2, dtype=np.float32)[hh][None], (128, 1))),
    }
    for l in range(2):
        gains = np.concatenate([np.tile(inp['q_norm_g'][l], 4), inp['k_norm_g'][l]])
        lay = {
            "w_mod": inp['w_mod'][l], "b_mod2": np.tile(inp['b_mod'][l][None], (2, 1)),
            "gmix2": np.tile(inp['norm_mix_g'][l][None], (2, 1)), "gffn2": np.tile(inp['norm_ffn_g'][l][None], (2, 1)),
            "gains320": np.tile(gains[None], (128, 1)), "w_out": inp['w_out'][l], "router_w": inp['router_w'][l],
            "router_b128": np.tile(inp['router_b'][l][None], (128, 1)), "e_w1": inp['e_w1'][l],
            "e_b1T": inp['e_b1'][l].reshape(32, 16, 128).transpose(0, 2, 1), "e_w2": inp['e_w2'][l], "e_b2": inp['e_b2'][l],
        }
        for k, v in lay.items():
            m[f"{k}_l{l}"] = np.ascontiguousarray(v, dtype=np.float32)
        w_in = inp['w_in'][l]
        sl = lambda a, n: w_in[:, a:a + n]
        for h2 in range(2):
            w_in_c = np.concatenate([
                sl(1536 + h2 * 256, 256), sl(h2 * 64, 64), sl(128 + h2 * 64, 64),
                sl(256 + h2 * 256, 256), sl(768 + h2 * 256, 256), sl(2048 + h2 * 256, 256),
                sl(1280, 128), sl(1408, 128), sl(2560, 128)], axis=1)
            m[f"w_in_c_l{l}h{h2}"] = np.ascontiguousarray(w_in_c)
            for k, v in rwkv_inputs(l, h2, inp).items():
                if k != "rconsts":
                    m[f"{k}_l{l}h{h2}"] = v
    return m


_NC_CACHE = {}


def run_fused(inp, T, n_exp=32):
    inp = {k: np.asarray(v) for k, v in inp.items()}
    Bn = inp['x'].shape[0]
    H = T // 2
    key = ('fused', T, n_exp)
    if key not in _NC_CACHE:
        _NC_CACHE[key] = build_fused(T, n_exp)
    cores = list(range(2 * Bn))
    res = run_bass_kernel_spmd(_NC_CACHE[key], [fused_inputs(T, c // 2, c % 2, inp) for c in cores], core_ids=cores).results
    out = np.empty((Bn, T, D), np.float32)
    for c in cores:
        out[c // 2, (c % 2) * H:(c % 2 + 1) * H] = np.asarray(res[c]['xfin'])
    return out


def kernel(**inputs):
    return run_fused(inputs, 8192)
```

```python
import numpy as np
from contextlib import ExitStack
import concourse.bass as bass
import concourse.mybir as mybir
from concourse.bass_utils import run_bass_kernel_spmd

F32 = mybir.dt.float32
BF16 = mybir.dt.bfloat16
AF = mybir.ActivationFunctionType
ALU = mybir.AluOpType
AX = mybir.AxisListType

D = 1024
CTX = 256
NDS = 8
EPS = 1e-6


class Buf:
    __slots__ = ("t", "w", "r")

    def __init__(self, t):
        self.t = t
        self.w = None
        self.r = {}

    def __getitem__(self, k):
        return self.t[k]


class KB:
    def __init__(self):
        self.nc = bass.Bass("TRN2", target_bir_lowering=False)
        self.es = ExitStack()
        nc = self.nc
        self.eng = {'pe': nc.tensor, 'act': nc.scalar, 'dve': nc.vector, 'pool': nc.gpsimd, 'sp': nc.sync}
        self.sem = {k: self.es.enter_context(nc.semaphore("s_" + k)) for k in self.eng}
        self.cnt = {k: 0 for k in self.eng}
        self.waited = {k: {} for k in self.eng}
        self.dsem = {}
        for q in ('sp', 'pool', 'act'):
            self.dsem[q] = [[self.es.enter_context(nc.semaphore(f"d_{q}{i}")), 0] for i in range(NDS)]
        self.dptr = {q: 0 for q in self.dsem}
        self.nbuf = 0
        self.override = {}
        self.sfx = {}
        self.registry = {}
        self.banks = [Buf(self.es.enter_context(nc.psum_tensor(f"bank{i}", [128, 512], F32))) for i in range(8)]

    def sb(self, shape, dt, name=None):
        self.nbuf += 1
        return Buf(self.es.enter_context(self.nc.sbuf_tensor(name or f"sb{self.nbuf}", list(shape), dt)))

    def ring(self, n, shape, dt, name=None):
        return [self.sb(shape, dt, None if name is None else f"{name}{i}") for i in range(n)]

    def dram(self, name, shape, dt, kind="Internal"):
        if name in self.override:
            return self.override[name]
        full = name + self.sfx.get(name, "")
        if full not in self.registry:
            self.registry[full] = self.nc.dram_tensor(full, list(shape), dt, kind=kind).ap()
        return self.registry[full]

    limit = None
    nops = 0

    def op(self, e, fn, reads=(), writes=(), dma=False):
        self.nops += 1
        if self.limit is not None and self.nops > self.limit:
            return None
        deps = {}

        def add(tok):
            if tok is None:
                return
            key, sem, val = tok
            if key not in deps or deps[key][1] < val:
                deps[key] = (sem, val)

        for b in reads:
            add(b.w)
        for b in writes:
            add(b.w)
            for tok in b.r.values():
                add(tok)
        eng = self.eng[e]
        wt = self.waited[e]
        for key, (sem, val) in deps.items():
            if key == e and e == 'pe':
                continue
            if wt.get(key, 0) >= val:
                continue
            eng.wait_ge(sem, val)
            wt[key] = val
        if dma:
            ring = self.dsem[e]
            i = self.dptr[e]
            key = f"d_{e}{i}"
            if ring[i][1] and wt.get(key, 0) < ring[i][1]:
                eng.wait_ge(ring[i][0], ring[i][1])
                wt[key] = ring[i][1]
        inst = fn(eng)
        if dma:
            self.dptr[e] = (i + 1) % len(ring)
            ring[i][1] += 16
            inst.then_inc(ring[i][0], 16)
            tok = (f"d_{e}{i}", ring[i][0], ring[i][1])
        else:
            self.cnt[e] += 1
            inst.then_inc(self.sem[e], 1)
            tok = (e, self.sem[e], self.cnt[e])
        for b in reads:
            b.r[tok[0]] = tok
        for b in writes:
            b.w = tok
            b.r = {}
        return tok

    def barrier(self):
        for e, eng in self.eng.items():
            wt = self.waited[e]
            for k in self.eng:
                if k != e and self.cnt[k] and wt.get(k, 0) < self.cnt[k]:
                    eng.wait_ge(self.sem[k], self.cnt[k])
                    wt[k] = self.cnt[k]
            for q, ring in self.dsem.items():
                for i, (sem, val) in enumerate(ring):
                    key = f"d_{q}{i}"
                    if val and wt.get(key, 0) < val:
                        eng.wait_ge(sem, val)
                        wt[key] = val

    def push(self):
        self._saved = getattr(self, '_saved', [])
        self._saved.append(self.es)
        self.es = ExitStack()

    def pop(self):
        self.barrier()
        self.es.close()
        self.es = self._saved.pop()

    def finish(self):
        sp = self.eng['sp']
        for q, ring in self.dsem.items():
            for sem, val in ring:
                if val:
                    sp.wait_ge(sem, val)
        for k in self.eng:
            if self.cnt[k]:
                sp.wait_ge(self.sem[k], self.cnt[k])
        self.es.close()


C_ID = 0
C_SEL65 = 128
C_SEL2 = 192
C_W = 448


def make_consts():
    c = np.zeros((128, C_W), np.float32)
    c[:, C_ID:C_ID + 128] = np.eye(128, dtype=np.float32)
    c[64, C_SEL65:C_SEL65 + 64] = 1.0
    c[0, C_SEL2:C_SEL2 + 128] = 1.0
    c[1, C_SEL2 + 128:C_SEL2 + 256] = 1.0
    return c


def rope_tables(T):
    row = np.repeat(np.arange(T // 64), 64)
    col = np.tile(np.arange(64), T // 64)
    pos = np.stack([row, col], axis=-1).astype(np.float32)
    freqs = (np.float32(10000.0) ** (-np.arange(16, dtype=np.float32) / np.float32(16))).astype(np.float32)
    ang = (pos[:, :, None] * freqs).astype(np.float32)
    return np.cos(ang).reshape(T, 32).astype(np.float32), np.sin(ang).reshape(T, 32).astype(np.float32)


def phase_mod(kb, cst, identF, cc_d, wmod_d, bmod2_d, g2a_d, g2b_d, want_bc):
    nc = kb.nc
    bA, bB = kb.banks[0], kb.banks[1]
    fm = kb.sb([128, 4, 8, 2], F32)
    bc = {}
    if want_bc:
        for name in ('gm', 'gf'):
            for r in range(2):
                bc[(name, r)] = kb.sb([128, 1024], F32)
    kb.push()
    cc16 = kb.sb([16, 128], F32)
    kb.op('sp', lambda e: e.dma_start(out=cc16[:], in_=cc_d.rearrange("r (k p) -> (r k) p", p=128)), writes=[cc16], dma=True)
    bmod2 = kb.sb([2, 6144], F32)
    kb.op('sp', lambda e: e.dma_start(out=bmod2[:], in_=bmod2_d), writes=[bmod2], dma=True)
    g2a = kb.sb([2, 1024], F32)
    g2b = kb.sb([2, 1024], F32)
    kb.op('sp', lambda e: e.dma_start(out=g2a[:], in_=g2a_d), writes=[g2a], dma=True)
    kb.op('sp', lambda e: e.dma_start(out=g2b[:], in_=g2b_d), writes=[g2b], dma=True)
    kb.op('pe', lambda e: e.transpose(out=bA[:, 0:16], in_=cc16[:], identity=identF[0:16, 0:16]), reads=[cc16, identF], writes=[bA])
    siluT = kb.sb([128, 16], F32)
    kb.op('act', lambda e: e.activation(out=siluT[:], in_=bA[:, 0:16], func=AF.Silu), reads=[bA], writes=[siluT])
    sview = siluT[:].rearrange("p (r k) -> p k r", k=8)
    wm = kb.ring(2, [128, 8, 512], F32)
    mod_row = kb.sb([2, 6144], F32)
    wsrc = wmod_d.rearrange("(k p) c -> p k c", p=128)
    for cch in range(12):
        w = wm[cch % 2]
        c0 = cch * 512
        kb.op('sp', lambda e: e.dma_start(out=w[:], in_=wsrc[:, :, c0:c0 + 512]), writes=[w], dma=True)
        bank = (bA, bB)[cch % 2]
        for k in range(8):
            kb.op('pe', lambda e: e.matmul(bank[0:2, 0:512], lhsT=sview[:, k, :], rhs=w[:, k, :], start=(k == 0), stop=(k == 7)),
                  reads=[siluT, w], writes=[bank])
        kb.op('dve', lambda e: e.tensor_tensor(out=mod_row[:, c0:c0 + 512], in0=bank[0:2, 0:512], in1=bmod2[:, c0:c0 + 512], op=ALU.add),
              reads=[bank, bmod2], writes=[mod_row])
    Am = kb.sb([2, 1024], F32)
    Af = kb.sb([2, 1024], F32)
    kb.op('dve', lambda e: e.scalar_tensor_tensor(out=Am[:], in0=mod_row[:, 1024:2048], scalar=1.0, in1=g2a[:], op0=ALU.add, op1=ALU.mult),
          reads=[mod_row, g2a], writes=[Am])
    kb.op('dve', lambda e: e.scalar_tensor_tensor(out=Af[:], in0=mod_row[:, 4096:5120], scalar=1.0, in1=g2b[:], op0=ALU.add, op1=ALU.mult),
          reads=[mod_row, g2b], writes=[Af])
    srcs = [(Am, 0), (mod_row, 0), (Af, 0), (mod_row, 3072)]
    for qi, (src, off) in enumerate(srcs):
        for k in range(8):
            col = (qi * 8 + k) * 2
            kb.op('pe', lambda e: e.transpose(out=bA[:, col:col + 2], in_=src[0:2, off + k * 128: off + (k + 1) * 128], identity=identF[0:2, 0:2]),
                  reads=[src, identF], writes=[bA])
    kb.op('dve', lambda e: e.tensor_copy(out=fm[:].rearrange("p a k r -> p (a k r)"), in_=bA[:, 0:64]), reads=[bA], writes=[fm])
    if want_bc:
        sel2 = kb.sb([2, 256], F32)
        kb.op('sp', lambda e: e.dma_start(out=sel2[:], in_=cst[0:2, C_SEL2:C_SEL2 + 256]), writes=[sel2], dma=True)
        for name, off in (('gm', 2048), ('gf', 5120)):
            for r in range(2):
                t = bc[(name, r)]
                for hf in range(2):
                    bank = (bA, bB)[hf]
                    kb.op('pe', lambda e: e.matmul(bank[:, 0:512], lhsT=sel2[:, r * 128:(r + 1) * 128], rhs=mod_row[:, off + hf * 512: off + (hf + 1) * 512], start=True, stop=True),
                          reads=[sel2, mod_row], writes=[bank])
                    kb.op('dve', lambda e: e.tensor_copy(out=t[:, hf * 512:(hf + 1) * 512], in_=bank[:, 0:512]), reads=[bank], writes=[t])
    kb.pop()
    return None, fm, bc


def build_mix(T, with_ctx_q, debug=False, stop_after=None, kb=None, q_half=False):
    own = kb is None
    if own:
        kb = KB()
    nc = kb.nc
    kb.push()
    NTOK = CTX + T
    NT = NTOK // 128
    xin = kb.dram("xin", [NTOK, D], F32, "ExternalInput")
    cc_d = kb.dram("cc", [2, D], F32, "ExternalInput")
    wmod_d = kb.dram("w_mod", [D, 6 * D], F32, "ExternalInput")
    bmod2_d = kb.dram("b_mod2", [2, 6 * D], F32, "ExternalInput")
    g2a_d = kb.dram("gmix2", [2, D], F32, "ExternalInput")
    g2b_d = kb.dram("gffn2", [2, D], F32, "ExternalInput")
    win_d = kb.dram("w_in_c", [D, 1536], F32, "ExternalInput")
    gains_d = kb.dram("gains320", [128, 320], F32, "ExternalInput")
    cos_d = kb.dram("rope_cos", [T, 32], F32, "ExternalInput")
    sin_d = kb.dram("rope_sin", [T, 32], F32, "ExternalInput")
    cst = kb.dram("consts", [128, C_W], F32, "ExternalInput")
    oat_d = kb.dram("oat", [4, 64, T], BF16, "ExternalOutput")
    oatc_d = kb.dram("oatc", [4, 64, CTX], BF16, "ExternalOutput")
    rwp_d = kb.dram("rwproj", [9, 128, NTOK], F32, "ExternalOutput" if debug else "Internal")

    identF = kb.sb([128, 128], F32)
    kb.op('sp', lambda e: e.dma_start(out=identF[:], in_=cst[:, C_ID:C_ID + 128]), writes=[identF], dma=True)
    identB = kb.sb([128, 128], BF16)
    kb.op('dve', lambda e: e.tensor_copy(out=identB[:], in_=identF[:]), reads=[identF], writes=[identB])
    sel65 = kb.sb([65, 64], F32)
    kb.op('sp', lambda e: e.dma_start(out=sel65[:], in_=cst[0:65, C_SEL65:C_SEL65 + 64]), writes=[sel65], dma=True)

    mod_row, fm, _ = phase_mod(kb, cst, identF, cc_d, wmod_d, bmod2_d, g2a_d, g2b_d, False)

    kb.push()
    Wc = kb.sb([128, 8, 1536], BF16)
    wsrc = win_d.rearrange("(k p) c -> p k c", p=128)
    for k in range(8):
        kb.op('pool', lambda e: e.dma_start(out=Wc[:, k, :], in_=wsrc[:, k, :]), writes=[Wc], dma=True)
    gains = kb.sb([128, 320], F32)
    kb.op('sp', lambda e: e.dma_start(out=gains[:], in_=gains_d), writes=[gains], dma=True)

    QT = kb.sb([64, 4, T], BF16)
    QTc = kb.sb([64, 4, CTX], BF16)
    KT = kb.sb([64, NTOK], BF16)
    V1 = kb.sb([128, NT, 65], BF16)
    kb.op('pool', lambda e: e.memset(V1[:, :, 64:65], 1.0), writes=[V1])

    x_ring = kb.ring(3, [128, D], F32)
    junk = kb.sb([128, D], BF16)
    xn_ring = kb.ring(2, [128, D], BF16)
    st_ring = kb.ring(2, [128, 8], F32)
    nT_ring = kb.ring(2, [128, 8, 512], BF16)
    qkv_ring = kb.ring(2, [128, 384], F32)
    tmpa = kb.ring(2, [128, 320], F32)
    tmpb = kb.ring(2, [128, 320], F32)
    qr_ring = kb.ring(2, [128, 320], BF16)
    cs_ring = kb.ring(2, [128, 64], F32)
    fmsb = kb.ring(3, [128, 512], F32)
    B = kb.banks
    b_tp, b_tm, b_q, b_fm = B[2], B[3], B[4], (B[5], B[6])

    groups = [(0, 2, True)] + [(2 + 4 * g, 4, False) for g in range(T // 512)]
    fmcount = 0
    for gi, (t0, nt, isctx) in enumerate(groups):
        r = 1 if isctx else 0
        nTb = nT_ring[gi % 2]
        ntok = nt * 128
        for i in range(nt):
            ti = t0 + i
            xt = x_ring[ti % 3]
            xn = xn_ring[ti % 2]
            st = st_ring[ti % 2]
            kb.op('sp', lambda e: e.dma_start(out=xt[:], in_=xin[ti * 128:(ti + 1) * 128, :]), writes=[xt], dma=True)
            kb.op('act', lambda e: e.activation(out=junk[:], in_=xt[:], func=AF.Square, accum_out=st[:, 0:1]), reads=[xt], writes=[junk, st])
            kb.op('dve', lambda e: e.tensor_scalar(out=st[:, 1:2], in0=st[:, 0:1], scalar1=1.0 / D, scalar2=EPS, op0=ALU.mult, op1=ALU.add), reads=[st], writes=[st])
            kb.op('act', lambda e: e.activation(out=st[:, 1:2], in_=st[:, 1:2], func=AF.Sqrt), reads=[st], writes=[st])
            kb.op('dve', lambda e: e.reciprocal(out=st[:, 2:3], in_=st[:, 1:2]), reads=[st], writes=[st])
            kb.op('act', lambda e: e.activation(out=xn[:], in_=xt[:], func=AF.Copy, scale=st[:, 2:3]), reads=[xt, st], writes=[xn])
            tpb = b_tp[:].bitcast(BF16)
            for k in range(8):
                kb.op('pe', lambda e: e.transpose(out=tpb[:, k * 128:(k + 1) * 128], in_=xn[:, k * 128:(k + 1) * 128], identity=identB[:]),
                      reads=[xn, identB], writes=[b_tp])
            for k in range(8):
                kb.op('act', lambda e: e.activation(out=nTb[:, k, i * 128:(i + 1) * 128], in_=tpb[:, k * 128:(k + 1) * 128], func=AF.Identity,
                                                    scale=fm[:, 0, k, r:r + 1], bias=fm[:, 1, k, r:r + 1]), reads=[b_tp, fm], writes=[nTb])
        for i in range(nt):
            ti = t0 + i
            for k in range(8):
                kb.op('pe', lambda e: e.matmul(b_tm[:, 0:384], lhsT=nTb[:, k, i * 128:(i + 1) * 128], rhs=Wc[:, k, 0:384], start=(k == 0), stop=(k == 7)),
                      reads=[nTb, Wc], writes=[b_tm])
            qkv = qkv_ring[ti % 2]
            ta, tb2, qr, st = tmpa[ti % 2], tmpb[ti % 2], qr_ring[ti % 2], st_ring[ti % 2]
            kb.op('act', lambda e: e.copy(out=qkv[:], in_=b_tm[:, 0:384]), reads=[b_tm], writes=[qkv])
            kb.op('pool', lambda e: e.tensor_copy(out=V1[:, ti, 0:64], in_=qkv[:, 320:384]), reads=[qkv], writes=[V1])
            kb.op('dve', lambda e: e.tensor_tensor(out=ta[:], in0=qkv[:, 0:320], in1=qkv[:, 0:320], op=ALU.mult), reads=[qkv], writes=[ta])
            kb.op('dve', lambda e: e.tensor_reduce(out=st[:, 3:8], in_=ta[:].rearrange("p (h d) -> p h d", d=64), axis=AX.X, op=ALU.add), reads=[ta], writes=[st])
            kb.op('dve', lambda e: e.tensor_scalar(out=st[:, 3:8], in0=st[:, 3:8], scalar1=1.0 / 64, scalar2=EPS, op0=ALU.mult, op1=ALU.add), reads=[st], writes=[st])
            kb.op('act', lambda e: e.activation(out=st[:, 3:8], in_=st[:, 3:8], func=AF.Sqrt), reads=[st], writes=[st])
            kb.op('dve', lambda e: e.reciprocal(out=st[:, 3:8], in_=st[:, 3:8]), reads=[st], writes=[st])
            kb.op('dve', lambda e: e.tensor_tensor(out=ta[:].rearrange("p (h d) -> p h d", d=64), in0=qkv[:, 0:320].rearrange("p (h d) -> p h d", d=64),
                                                   in1=st[:, 3:8].unsqueeze(2).broadcast_to([128, 5, 64]), op=ALU.mult), reads=[qkv, st], writes=[ta])
            if isctx:
                kb.op('dve', lambda e: e.tensor_tensor(out=qr[:], in0=ta[:], in1=gains[:], op=ALU.mult), reads=[ta, gains], writes=[qr])
            else:
                kb.op('dve', lambda e: e.tensor_tensor(out=tb2[:], in0=ta[:], in1=gains[:], op=ALU.mult), reads=[ta, gains], writes=[tb2])
                cs = cs_ring[ti % 2]
                tl = (ti - 2) * 128
                kb.op('sp', lambda e: e.dma_start(out=cs[:, 0:32], in_=cos_d[tl:tl + 128, :]), writes=[cs], dma=True)
                kb.op('sp', lambda e: e.dma_start(out=cs[:, 32:64], in_=sin_d[tl:tl + 128, :]), writes=[cs], dma=True)
                xv = tb2[:].rearrange("p (h a w q) -> p h a w q", h=5, a=2, w=2, q=16)
                ov = qr[:].rearrange("p (h a w q) -> p h a w q", h=5, a=2, w=2, q=16)
                tv = ta[:].rearrange("p (h a w q) -> p h a w q", h=5, a=2, w=2, q=16)
                cv = cs[:, 0:32].rearrange("p (a q) -> p a q", a=2).unsqueeze(1).broadcast_to([128, 5, 2, 16])
                sv = cs[:, 32:64].rearrange("p (a q) -> p a q", a=2).unsqueeze(1).broadcast_to([128, 5, 2, 16])
                x1, x2 = xv[:, :, :, 0, :], xv[:, :, :, 1, :]
                kb.op('dve', lambda e: e.tensor_tensor(out=tv[:, :, :, 0, :], in0=x2, in1=sv, op=ALU.mult), reads=[tb2, cs], writes=[ta])
                kb.op('dve', lambda e: e.tensor_tensor(out=tv[:, :, :, 1, :], in0=x1, in1=sv, op=ALU.mult), reads=[tb2, cs], writes=[ta])
                kb.op('dve', lambda e: e.tensor_tensor(out=x1, in0=x1, in1=cv, op=ALU.mult), reads=[tb2, cs, ta], writes=[tb2])
                kb.op('dve', lambda e: e.tensor_tensor(out=x2, in0=x2, in1=cv, op=ALU.mult), reads=[tb2, cs], writes=[tb2])
                kb.op('dve', lambda e: e.tensor_tensor(out=ov[:, :, :, 0, :], in0=x1, in1=tv[:, :, :, 0, :], op=ALU.subtract), reads=[tb2, ta], writes=[qr])
                kb.op('dve', lambda e: e.tensor_tensor(out=ov[:, :, :, 1, :], in0=x2, in1=tv[:, :, :, 1, :], op=ALU.add), reads=[tb2, ta], writes=[qr])
            qb = b_q[:].bitcast(BF16)
            for h in range(5):
                kb.op('pe', lambda e: e.transpose(out=qb[0:64, h * 128:(h + 1) * 128], in_=qr[:, h * 64:(h + 1) * 64], identity=identB[:]),
                      reads=[qr, identB], writes=[b_q])
            if isctx:
                if with_ctx_q:
                    kb.op('act', lambda e: e.copy(out=QTc[:, :, ti * 128:(ti + 1) * 128], in_=qb[0:64, 0:512].rearrange("p (h t) -> p h t", h=4)), reads=[b_q], writes=[QTc])
            else:
                tl = (ti - 2) * 128
                kb.op('act', lambda e: e.copy(out=QT[:, :, tl:tl + 128], in_=qb[0:64, 0:512].rearrange("p (h t) -> p h t", h=4)), reads=[b_q], writes=[QT])
            kb.op('act', lambda e: e.copy(out=KT[:, ti * 128:(ti + 1) * 128], in_=qb[0:64, 512:640]), reads=[b_q], writes=[KT])
        for j in range(9):
            bank = b_fm[fmcount % 2]
            sbt = fmsb[fmcount % 3]
            for k in range(8):
                kb.op('pe', lambda e: e.matmul(bank[:, 0:ntok], lhsT=Wc[:, k, 384 + j * 128: 384 + (j + 1) * 128], rhs=nTb[:, k, 0:ntok], start=(k == 0), stop=(k == 7)),
                      reads=[Wc, nTb], writes=[bank])
            if fmcount % 2 == 0:
                kb.op('act', lambda e: e.copy(out=sbt[:, 0:ntok], in_=bank[:, 0:ntok]), reads=[bank], writes=[sbt])
            else:
                kb.op('dve', lambda e: e.tensor_copy(out=sbt[:, 0:ntok], in_=bank[:, 0:ntok]), reads=[bank], writes=[sbt])
            kb.op('pool', lambda e: e.dma_start(out=rwp_d[j, :, t0 * 128: t0 * 128 + ntok], in_=sbt[:, 0:ntok]), reads=[sbt], dma=True)
            fmcount += 1

    if stop_after == 'proj':
        kb.pop()
        kb.pop()
        kb.finish()
        return nc
    bS = (B[0], B[1], B[2])
    bO = (B[3], B[4])
    bB = B[5]
    pT_ring = kb.ring(3, [128, 512], BF16)
    osb_ring = kb.ring(2, [65, 512], F32)
    rl_ring = kb.ring(2, [64, 512], F32)
    ot_ring = kb.ring(2, [64, 512], BF16)
    jobs = []
    if with_ctx_q:
        for h in range(4):
            jobs.append((QTc, h, 0, CTX, 2, oatc_d))
    TQ = T
    if q_half:
        TQ = T // 2
        msel_d = kb.dram("msel", [128, 2], F32, "ExternalInput")
        msel = kb.sb([128, 2], F32)
        kb.op('sp', lambda e: e.dma_start(out=msel[:], in_=msel_d), writes=[msel], dma=True)
        for h in range(4):
            kb.op('dve', lambda e: e.tensor_scalar(out=QT[:, h, 0:TQ], in0=QT[:, h, 0:TQ], scalar1=msel[0:64, 0:1], scalar2=None, op0=ALU.mult), reads=[QT, msel], writes=[QT])
            kb.op('dve', lambda e: e.scalar_tensor_tensor(out=QT[:, h, 0:TQ], in0=QT[:, h, TQ:T], scalar=msel[0:64, 1:2], in1=QT[:, h, 0:TQ], op0=ALU.mult, op1=ALU.add), reads=[QT, msel], writes=[QT])
    qblk = min(512, TQ)
    for qb_i in range(TQ // qblk):
        for h in range(4):
            jobs.append((QT, h, qb_i * qblk, qblk, NT, oat_d))
    sc = 0
    for ji, (Qsrc, h, q0, nq, nkt, dst) in enumerate(jobs):
        pso = bO[ji % 2]

        def issue_S(kt, sidx):
            bank = bS[sidx % 3]
            kb.op('pe', lambda e: e.matmul(bank[:, 0:nq], lhsT=KT[:, kt * 128:(kt + 1) * 128], rhs=Qsrc[:, h, q0:q0 + nq], start=True, stop=True),
                  reads=[KT, Qsrc], writes=[bank])
        issue_S(0, sc)
        if nkt > 1:
            issue_S(1, sc + 1)
        for kt in range(nkt):
            bank = bS[(sc + kt) % 3]
            pT = pT_ring[(sc + kt) % 3]
            kb.op('act', lambda e: e.activation(out=pT[:, 0:nq], in_=bank[:, 0:nq], func=AF.Exp, scale=0.125), reads=[bank], writes=[pT])
            if kt + 2 < nkt:
                issue_S(kt + 2, sc + kt + 2)
            kb.op('pe', lambda e: e.matmul(pso[0:65, 0:nq], lhsT=V1[:, kt, 0:65], rhs=pT[:, 0:nq], start=(kt == 0), stop=(kt == nkt - 1)),
                  reads=[V1, pT], writes=[pso])
        sc += nkt
        osb, rl, ot = osb_ring[ji % 2], rl_ring[ji % 2], ot_ring[ji % 2]
        kb.op('dve', lambda e: e.tensor_copy(out=osb[:, 0:nq], in_=pso[0:65, 0:nq]), reads=[pso], writes=[osb])
        kb.op('pe', lambda e: e.matmul(bB[0:64, 0:nq], lhsT=sel65[:], rhs=osb[:, 0:nq], start=True, stop=True), reads=[sel65, osb], writes=[bB])
        kb.op('dve', lambda e: e.reciprocal(out=rl[:, 0:nq], in_=bB[0:64, 0:nq]), reads=[bB], writes=[rl])
        kb.op('dve', lambda e: e.tensor_tensor(out=ot[:, 0:nq], in0=osb[0:64, 0:nq], in1=rl[:, 0:nq], op=ALU.mult), reads=[osb, rl], writes=[ot])
        kb.op('pool', lambda e: e.dma_start(out=dst[h, :, q0:q0 + nq], in_=ot[:, 0:nq]), reads=[ot], dma=True)
    kb.pop()
    if stop_after == 'attn':
        kb.pop()
        kb.finish()
        return nc
    phase_rwkv(kb, T, with_ctx_q, identF, rwp_d, debug)
    kb.pop()
    if own:
        kb.finish()
    return nc


def mix_inputs(T, l, b, hh, xl, xc, inp):
    w_in = inp['w_in'][l]
    sl = lambda a, n: w_in[:, a:a + n]
    w_in_c = np.concatenate([
        sl(1536 + hh * 256, 256), sl(hh * 64, 64), sl(128 + hh * 64, 64),
        sl(256 + hh * 256, 256), sl(768 + hh * 256, 256), sl(2048 + hh * 256, 256),
        sl(1280, 128), sl(1408, 128), sl(2560, 128)], axis=1)
    gains = np.concatenate([np.tile(inp['q_norm_g'][l], 4), inp['k_norm_g'][l]])
    cos, sin = rope_tables(T)
    return {
        "xin": np.ascontiguousarray(np.concatenate([xc[b], xl[b]], axis=0)),
        "cc": np.ascontiguousarray(np.stack([inp['c'][b], inp['c_ctx']])),
        "w_mod": np.ascontiguousarray(inp['w_mod'][l]),
        "b_mod2": np.ascontiguousarray(np.tile(inp['b_mod'][l][None], (2, 1))),
        "gmix2": np.ascontiguousarray(np.tile(inp['norm_mix_g'][l][None], (2, 1))),
        "gffn2": np.ascontiguousarray(np.tile(inp['norm_ffn_g'][l][None], (2, 1))),
        "w_in_c": np.ascontiguousarray(w_in_c),
        "gains320": np.ascontiguousarray(np.tile(gains[None], (128, 1))),
        "rope_cos": cos, "rope_sin": sin,
        "consts": make_consts(),
        **rwkv_inputs(l, hh, inp),
    }


RC_BONES = 0
RC_BONES64 = 128
RC_TRI_F = 256
RC_TRI_B = 512
RC_MASK_F = 768
RC_MASK_B = 1280
RC_W = 1792
GN_EPS = 64e-5
LCH = 64


def make_rconsts():
    c = np.zeros((128, RC_W), np.float32)
    bd = np.kron(np.eye(2, dtype=np.float32), np.ones((64, 64), np.float32))
    c[:, RC_BONES:RC_BONES + 128] = bd
    c[:, RC_BONES64:RC_BONES64 + 128] = bd / 64.0
    s = np.arange(64)[:, None]
    t = np.arange(64)[None, :]
    cdec = -np.exp(np.float32(-0.5))
    for off, incl, excl in ((RC_TRI_F, s <= t, s < t), (RC_TRI_B, s >= t, s > t)):
        blk = np.concatenate([incl, excl], axis=1).astype(np.float32) * cdec
        c[0:64, off:off + 128] = blk
        c[64:128, off + 128:off + 256] = blk
    e2 = np.eye(2, dtype=np.float32)
    SU = np.kron(e2, (s < t).astype(np.float32))
    IU = np.kron(e2, (s <= t).astype(np.float32))
    SL = np.kron(e2, (s > t).astype(np.float32))
    IL = np.kron(e2, (s >= t).astype(np.float32))
    c[:, RC_MASK_F:RC_MASK_F + 512] = np.concatenate([SU, IU, SL, SL], axis=1)
    c[:, RC_MASK_B:RC_MASK_B + 512] = np.concatenate([SL, IL, SU, SU], axis=1)
    return c


def phase_rwkv(kb, T, with_ctx, identF, rwp_d, debug):
    NTOK = CTX + T
    kb.push()
    cw_d = kb.dram("rw_cw", [128, 18], F32, "ExternalInput")
    wupA_d = kb.dram("rw_wupA", [2, 65, 256], F32, "ExternalInput")
    aupA_d = kb.dram("rw_aupA", [2, 65, 256], F32, "ExternalInput")
    gup_d = kb.dram("rw_gup", [128, 256], F32, "ExternalInput")
    cv_d = kb.dram("rw_cv", [128, 10], F32, "ExternalInput")
    rc_d = kb.dram("rconsts", [128, RC_W], F32, "ExternalInput")
    orw_d = kb.dram("orw", [4, 64, T], BF16, "ExternalOutput")
    orwc_d = kb.dram("orwc", [4, 64, CTX], BF16, "ExternalOutput")
    odir_d = kb.dram("odir", [2, 2, 128, NTOK], F32, "ExternalOutput" if debug else "Internal")

    def load(shape, src, q='sp'):
        t = kb.sb(shape, F32)
        kb.op(q, lambda e: e.dma_start(out=t[:], in_=src), writes=[t], dma=True)
        return t
    RC = load([128, RC_W], rc_d)
    convw = load([128, 18], cw_d)
    cv = load([128, 10], cv_d)
    gup = load([128, 256], gup_d)
    wupA = kb.sb([65, 2, 256], F32)
    aupA = kb.sb([65, 2, 256], F32)
    for d in range(2):
        kb.op('sp', lambda e: e.dma_start(out=wupA[:, d, :], in_=wupA_d[d]), writes=[wupA], dma=True)
        kb.op('sp', lambda e: e.dma_start(out=aupA[:, d, :], in_=aupA_d[d]), writes=[aupA], dma=True)
    dv = kb.sb([128, 2, 2], F32)
    for p in range(2):
        kb.op('dve', lambda e: e.tensor_scalar(out=dv[:, p, 0:1], in0=cv[:, p * 5 + 1:p * 5 + 2], scalar1=-1.0, scalar2=1.0, op0=ALU.mult, op1=ALU.add), reads=[cv], writes=[dv])
        kb.op('dve', lambda e: e.tensor_scalar(out=dv[:, p, 1:2], in0=cv[:, p * 5 + 1:p * 5 + 2], scalar1=0.5, scalar2=None, op0=ALU.mult), reads=[cv], writes=[dv])
    BONES = RC[:, RC_BONES:RC_BONES + 128]
    BONES64 = RC[:, RC_BONES64:RC_BONES64 + 128]
    maskBD4 = lambda nch: RC[:, RC_BONES:RC_BONES + 128].rearrange("p (h s) -> p h s", h=2).unsqueeze(1).broadcast_to([128, nch, 2, 64])

    B = kb.banks
    hctr = [0]

    def half():
        i = hctr[0] % 8
        hctr[0] += 1
        t = B[i].t
        return B[i], (lambda a, b, t=t: t[:, a:b])

    groups = [(0, CTX, True)] + [(CTX + 512 * g, 512, False) for g in range(T // 512)]

    kb.push()
    raw = kb.ring(3, [128, 514], F32)
    cq = kb.ring(3, [128, 512], F32)
    xwd = kb.sb([65, 512], F32)
    xad = kb.ring(2, [65, 512], F32)
    kb.op('pool', lambda e: e.memset(xwd[64:65, :], 1.0), writes=[xwd])
    for x_ in xad:
        kb.op('pool', lambda e: e.memset(x_[64:65, :], 1.0), writes=[x_])
    sg = kb.sb([128, 4, 128], F32)
    a_sb = kb.ring(2, [128, 512], F32)
    cw_sb = kb.sb([128, 8, 128], F32)
    Es = kb.ring(4, [128, 512], F32)
    dl = kb.sb([128, 512], F32)
    tA = kb.ring(6, [128, 512], F32)
    BDall = kb.ring(2, [128, 8, 6, 128], F32)
    BDV = kb.ring(2, [128, 8, 128], F32)
    E1keep = kb.ring(2, [128, 512], F32)
    NB = 4
    XR = kb.ring(NB, [128, 256], F32)
    YA = kb.ring(NB, [128, 256], F32)
    AK = kb.ring(NB, [128, 256], F32)
    Xp = [kb.ring(NB, [128, 128], F32) for _ in range(2)]
    Yp = [kb.ring(NB, [128, 128], F32) for _ in range(2)]
    Tp = [kb.ring(NB, [128, 128], F32) for _ in range(2)]
    AtT = kb.ring(NB, [128, 128], F32)
    BhT = kb.ring(NB, [128, 128], F32)
    Esb = kb.ring(NB, [128, 256], F32)
    VT = [kb.ring(NB, [128, 128], F32) for _ in range(2)]
    Qc = [kb.ring(NB, [128, 128], F32) for _ in range(2)]
    Mc = [kb.ring(NB, [128, 128], F32) for _ in range(2)]
    Fsb = [kb.ring(NB, [128, 256], F32) for _ in range(2)]
    ST = [kb.ring(2, [128, 128], F32) for _ in range(2)]
    ostage = kb.ring(2, [128, 512], F32)

    def conv(dst, src, col, ntok):
        kb.op('pool', lambda e: e.tensor_scalar(out=dst[:, 0:ntok], in0=src[:, 0:ntok], scalar1=convw[:, col:col + 1], scalar2=None, op0=ALU.mult), reads=[src, convw], writes=[dst])
        for tap in (1, 2):
            kb.op('dve', lambda e: e.scalar_tensor_tensor(out=dst[:, 0:ntok], in0=src[:, tap:tap + ntok], scalar=convw[:, col + tap:col + tap + 1], in1=dst[:, 0:ntok], op0=ALU.mult, op1=ALU.add),
                  reads=[src, convw, dst], writes=[dst])

    def load_raw(q, j, t0, ntok, ring=None):
        r_ = (ring or raw)[q]
        left_zero = (t0 == 0) or (t0 == CTX)
        right_zero = (t0 + ntok == CTX) or (t0 + ntok == NTOK)
        a = 1 if left_zero else 0
        b = ntok + 1 if right_zero else ntok + 2
        if left_zero:
            kb.op('pool', lambda e: e.memset(r_[:, 0:1], 0.0), writes=[r_])
        if right_zero:
            kb.op('pool', lambda e: e.memset(r_[:, ntok + 1:ntok + 2], 0.0), writes=[r_])
        kb.op('sp', lambda e: e.dma_start(out=r_[:, a:b], in_=rwp_d[j, :, t0 - 1 + a: t0 - 1 + b]), writes=[r_], dma=True)
        return r_

    def sig_a(dst, xa_t, d, p, t0, ntok):
        kb.op('sp', lambda e: e.dma_start(out=xa_t[0:64, 0:ntok], in_=rwp_d[7, d * 64:(d + 1) * 64, t0:t0 + ntok]), writes=[xa_t], dma=True)
        hb, hv = half()
        kb.op('pe', lambda e: e.matmul(hv(0, ntok), lhsT=aupA[:, d, p * 128:(p + 1) * 128], rhs=xa_t[:, 0:ntok], start=True, stop=True), reads=[aupA, xa_t], writes=[hb])
        kb.op('act', lambda e: e.activation(out=dst[:, 0:ntok], in_=hv(0, ntok), func=AF.Sigmoid), reads=[hb], writes=[dst])

    def prep(d, grp, p, slot):
        t0, ntok, isctx = grp
        nch = ntok // LCH
        ntile = ntok // 128
        TRI = RC_TRI_F if d == 0 else RC_TRI_B
        Lidx = LCH - 1 if d == 0 else 0
        bd = BDall[slot]
        v3 = lambda t: t[:, 0:ntok].rearrange("p (c s) -> p c s", s=LCH)
        for q, j in enumerate((0 + p, 2 + p, 4 + p)):
            r_ = load_raw(q, j, t0, ntok)
            conv(cq[q], r_, p * 9 + q * 3, ntok)
        k_, v_, r_c = cq
        kb.op('sp', lambda e: e.dma_start(out=xwd[0:64, 0:ntok], in_=rwp_d[6, d * 64:(d + 1) * 64, t0:t0 + ntok]), writes=[xwd], dma=True)
        kb.op('act', lambda e: e.activation(out=xwd[0:64, 0:ntok], in_=xwd[0:64, 0:ntok], func=AF.Tanh), reads=[xwd], writes=[xwd])
        hb, hv = half()
        for i in range(ntile):
            kb.op('pe', lambda e: e.matmul(hv(i * 128, (i + 1) * 128), lhsT=xwd[:, i * 128:(i + 1) * 128], rhs=wupA[:, d, p * 128:(p + 1) * 128], start=True, stop=True),
                  reads=[xwd, wupA], writes=[hb])
        kb.op('act', lambda e: e.activation(out=sg[:, 0:ntile, :], in_=hv(0, ntile * 128).rearrange("p (i c) -> p i c", c=128), func=AF.Sigmoid), reads=[hb], writes=[sg])
        cwb = [half() for _ in range((ntile + 1) // 2)]
        for i in range(ntile):
            hb, hv = cwb[i // 2]
            cc = (i % 2) * 256
            kb.op('pe', lambda e: e.matmul(hv(cc, cc + 256), lhsT=sg[:, i, :], rhs=RC[:, TRI:TRI + 256], start=True, stop=True),
                  reads=[sg, RC], writes=[hb])
        for bi in range((nch + 3) // 4):
            n_ = min(4, nch - bi * 4)
            hb, hv = cwb[bi]
            kb.op('act', lambda e: e.copy(out=cw_sb[:, bi * 4:bi * 4 + n_, :], in_=hv(0, n_ * 128).rearrange("p (c s) -> p c s", s=128)), reads=[hb], writes=[cw_sb])
        E1, E2, E3, EL = Es
        kb.op('dve', lambda e: e.tensor_tensor(out=v3(dl), in0=cw_sb[:, 0:nch, Lidx:Lidx + 1].broadcast_to([128, nch, LCH]), in1=cw_sb[:, 0:nch, 0:LCH], op=ALU.subtract), reads=[cw_sb], writes=[dl])
        kb.op('act', lambda e: e.activation(out=v3(E1), in_=cw_sb[:, 0:nch, 0:LCH], func=AF.Exp), reads=[cw_sb], writes=[E1])
        kb.op('act', lambda e: e.activation(out=v3(E2), in_=cw_sb[:, 0:nch, 0:LCH], func=AF.Exp, scale=-1.0), reads=[cw_sb], writes=[E2])
        kb.op('act', lambda e: e.activation(out=v3(E3), in_=cw_sb[:, 0:nch, LCH:2 * LCH], func=AF.Exp), reads=[cw_sb], writes=[E3])
        kb.op('act', lambda e: e.activation(out=EL[:, 0:ntok], in_=dl[:, 0:ntok], func=AF.Exp), reads=[dl], writes=[EL])
        kb.op('pool', lambda e: e.tensor_copy(out=E1keep[slot][:, 0:ntok], in_=E1[:, 0:ntok]), reads=[E1], writes=[E1keep[slot]])
        a_ = a_sb[0]
        sig_a(a_, xad[0], d, p, t0, ntok)
        kk, t1, kkn, u_, bop, t2 = tA
        N = slice(0, ntok)
        kb.op('dve', lambda e: e.tensor_scalar(out=kk[:, N], in0=k_[:, N], scalar1=cv[:, p * 5:p * 5 + 1], scalar2=None, op0=ALU.mult), reads=[k_, cv], writes=[kk])
        kb.op('pool', lambda e: e.tensor_tensor(out=t1[:, N], in0=kk[:, N], in1=kk[:, N], op=ALU.mult), reads=[kk], writes=[t1])
        hb, hv = half()
        kb.op('pe', lambda e: e.matmul(hv(0, ntok), lhsT=BONES, rhs=t1[:, N], start=True, stop=True), reads=[RC, t1], writes=[hb])
        kb.op('dve', lambda e: e.tensor_scalar(out=t2[:, N], in0=hv(0, ntok), scalar1=1e-24, scalar2=None, op0=ALU.max), reads=[hb], writes=[t2])
        kb.op('act', lambda e: e.activation(out=t2[:, N], in_=t2[:, N], func=AF.Sqrt), reads=[t2], writes=[t2])
        kb.op('dve', lambda e: e.reciprocal(out=t2[:, N], in_=t2[:, N]), reads=[t2], writes=[t2])
        kb.op('pool', lambda e: e.tensor_tensor(out=kkn[:, N], in0=kk[:, N], in1=t2[:, N], op=ALU.mult), reads=[kk, t2], writes=[kkn])
        kb.op('dve', lambda e: e.tensor_scalar(out=u_[:, N], in0=a_[:, N], scalar1=cv[:, p * 5 + 1:p * 5 + 2], scalar2=dv[:, p, 0:1], op0=ALU.mult, op1=ALU.add), reads=[a_, cv, dv], writes=[u_])
        kb.op('pool', lambda e: e.tensor_tensor(out=u_[:, N], in0=k_[:, N], in1=u_[:, N], op=ALU.mult), reads=[k_, u_], writes=[u_])
        kb.op('pool', lambda e: e.tensor_tensor(out=bop[:, N], in0=kkn[:, N], in1=a_[:, N], op=ALU.mult), reads=[kkn, a_], writes=[bop])
        kdir = u_

        def embed(oi, src):
            kb.op('dve', lambda e: e.tensor_tensor(out=bd[:, 0:nch, oi, :].rearrange("p c (h s) -> p c h s", h=2),
                                                   in0=v3(src).unsqueeze(2).broadcast_to([128, nch, 2, LCH]), in1=maskBD4(nch), op=ALU.mult), reads=[src, RC], writes=[bd])
        kb.op('dve', lambda e: e.scalar_tensor_tensor(out=t1[:, N], in0=kkn[:, N], scalar=-1.0, in1=E3[:, N], op0=ALU.mult, op1=ALU.mult), reads=[kkn, E3], writes=[t1])
        embed(0, t1)
        kb.op('pool', lambda e: e.tensor_tensor(out=t2[:, N], in0=r_c[:, N], in1=E1[:, N], op=ALU.mult), reads=[r_c, E1], writes=[t2])
        embed(1, t2)
        kb.op('pool', lambda e: e.tensor_tensor(out=t1[:, N], in0=bop[:, N], in1=E2[:, N], op=ALU.mult), reads=[bop, E2], writes=[t1])
        embed(2, t1)
        kb.op('pool', lambda e: e.tensor_tensor(out=t2[:, N], in0=kdir[:, N], in1=E2[:, N], op=ALU.mult), reads=[kdir, E2], writes=[t2])
        embed(3, t2)
        kb.op('pool', lambda e: e.tensor_tensor(out=t1[:, N], in0=bop[:, N], in1=EL[:, N], op=ALU.mult), reads=[bop, EL], writes=[t1])
        embed(4, t1)
        kb.op('pool', lambda e: e.tensor_tensor(out=t2[:, N], in0=kdir[:, N], in1=EL[:, N], op=ALU.mult), reads=[kdir, EL], writes=[t2])
        embed(5, t2)
        kb.op('dve', lambda e: e.tensor_tensor(out=BDV[slot][:, 0:nch, :].rearrange("p c (h s) -> p c h s", h=2),
                                               in0=v3(v_).unsqueeze(2).broadcast_to([128, nch, 2, LCH]), in1=maskBD4(nch), op=ALU.mult), reads=[v_, RC], writes=[BDV[slot]])

    ident = identF
    evc = [0]

    def evac_copy(dst_ap, src_ap, rd, wr):
        evc[0] += 1
        if evc[0] % 8:
            kb.op('act', lambda e: e.copy(out=dst_ap, in_=src_ap), reads=rd, writes=wr)
        else:
            kb.op('dve', lambda e: e.tensor_copy(out=dst_ap, in_=src_ap), reads=rd, writes=wr)

    def precompute(d, slot, chunks, bset):
        MASK = RC_MASK_F if d == 0 else RC_MASK_B
        bd = BDall[slot]
        nb = len(chunks)
        op_ = lambda ci, oi: bd[:, ci, oi, :]
        for bi, ci in enumerate(chunks):
            hb, hv = half()
            kb.op('pe', lambda e: e.matmul(hv(0, 256), lhsT=op_(ci, 2), rhs=bd[:, ci, 0:2, :].rearrange("p o s -> p (o s)"), start=True, stop=True), reads=[bd], writes=[hb])
            kb.op('dve', lambda e: e.tensor_tensor(out=XR[bi][:], in0=hv(0, 256), in1=RC[:, MASK:MASK + 256], op=ALU.mult), reads=[hb, RC], writes=[XR[bi]])
            hb, hv = half()
            kb.op('pe', lambda e: e.matmul(hv(0, 256), lhsT=op_(ci, 0), rhs=bd[:, ci, 2:4, :].rearrange("p o s -> p (o s)"), start=True, stop=True), reads=[bd], writes=[hb])
            kb.op('dve', lambda e: e.tensor_tensor(out=YA[bi][:], in0=hv(0, 256), in1=RC[:, MASK + 256:MASK + 512], op=ALU.mult), reads=[hb, RC], writes=[YA[bi]])
            hb, hv = half()
            kb.op('pe', lambda e: e.matmul(hv(0, 128), lhsT=op_(ci, 3), rhs=op_(ci, 1), start=True, stop=True), reads=[bd], writes=[hb])
            kb.op('dve', lambda e: e.tensor_tensor(out=AK[bi][:, 0:128], in0=hv(0, 128), in1=RC[:, MASK + 128:MASK + 256], op=ALU.mult), reads=[hb, RC], writes=[AK[bi]])
            kb.op('pool', lambda e: e.tensor_tensor(out=Tp[0][bi][:], in0=YA[bi][:, 0:128], in1=ident[:], op=ALU.add), reads=[YA[bi], ident], writes=[Tp[0][bi]])
        for bi, ci in enumerate(chunks):
            for src_ap, rd, dst, dcol in ((op_(ci, 0), bd, AtT[bi], 0), (op_(ci, 4), bd, BhT[bi], 0), (op_(ci, 5), bd, AK[bi], 128), (BDV[slot][:, ci, :], BDV[slot], VT[bset][bi], 0)):
                hb, hv = half()
                kb.op('pe', lambda e: e.transpose(out=hv(0, 128), in_=src_ap, identity=ident[:]), reads=[rd, ident], writes=[hb])
                evac_copy(dst[:, dcol:dcol + 128], hv(0, 128), [hb], [dst])
        Xc = [XR[bi][:, 0:128] for bi in range(nb)]
        Xb = [XR[bi] for bi in range(nb)]
        Yc = [YA[bi][:, 0:128] for bi in range(nb)]
        Yb = [YA[bi] for bi in range(nb)]
        tcur = 0
        for lvl in range(1, 6):
            pp = lvl % 2
            nX, nXb, nY, nYb = [], [], [], []
            for bi in range(nb):
                hb, hv = half()
                kb.op('pe', lambda e: e.matmul(hv(0, 128), lhsT=Yc[bi], rhs=Xc[bi], start=True, stop=True), reads=[Yb[bi], Xb[bi]], writes=[hb])
                evac_copy(Xp[pp][bi][:], hv(0, 128), [hb], [Xp[pp][bi]])
                nX.append(Xp[pp][bi][:]); nXb.append(Xp[pp][bi])
                if lvl < 5:
                    hb, hv = half()
                    kb.op('pe', lambda e: e.matmul(hv(0, 128), lhsT=Xc[bi], rhs=Yc[bi], start=True, stop=True), reads=[Yb[bi], Xb[bi]], writes=[hb])
                    evac_copy(Yp[pp][bi][:], hv(0, 128), [hb], [Yp[pp][bi]])
                    nY.append(Yp[pp][bi][:]); nYb.append(Yp[pp][bi])
            for bi in range(nb):
                hb, hv = half()
                told, tnew = Tp[tcur][bi], Tp[1 - tcur][bi]
                kb.op('pe', lambda e: e.matmul(hv(0, 128), lhsT=nX[bi], rhs=told[:], start=True, stop=True), reads=[nXb[bi], told], writes=[hb])
                kb.op('dve', lambda e: e.tensor_tensor(out=tnew[:], in0=hv(0, 128), in1=told[:], op=ALU.add), reads=[hb, told], writes=[tnew])
            tcur = 1 - tcur
            Xc, Xb, Yc, Yb = nX, nXb, nY, nYb
        for bi, ci in enumerate(chunks):
            Tt = Tp[tcur][bi]
            hb, hv = half()
            kb.op('pe', lambda e: e.matmul(hv(0, 128), lhsT=Tt[:], rhs=XR[bi][:, 128:256], start=True, stop=True), reads=[Tt, XR[bi]], writes=[hb])
            kb.op('pe', lambda e: e.matmul(hv(128, 256), lhsT=Tt[:], rhs=BhT[bi][:], start=True, stop=True), reads=[Tt, BhT[bi]], writes=[hb])
            evac_copy(Esb[bi][:], hv(0, 256), [hb], [Esb[bi]])
        for bi, ci in enumerate(chunks):
            hb, hv = half()
            kb.op('pe', lambda e: e.matmul(hv(0, 256), lhsT=AtT[bi][:], rhs=Esb[bi][:], start=True, stop=True), reads=[AtT[bi], Esb[bi]], writes=[hb])
            kb.op('dve', lambda e: e.tensor_tensor(out=Qc[bset][bi][:], in0=hv(0, 128), in1=op_(ci, 1), op=ALU.add), reads=[hb, bd], writes=[Qc[bset][bi]])
            wl_col = ci * LCH + (LCH - 1 if d == 0 else 0)
            kb.op('dve', lambda e: e.scalar_tensor_tensor(out=Mc[bset][bi][:], in0=ident[:], scalar=E1keep[slot][:, wl_col:wl_col + 1], in1=hv(128, 256), op0=ALU.mult, op1=ALU.add),
                  reads=[hb, ident, E1keep[slot]], writes=[Mc[bset][bi]])
            hb, hv = half()
            kb.op('pe', lambda e: e.matmul(hv(0, 256), lhsT=YA[bi][:, 128:256], rhs=Esb[bi][:], start=True, stop=True), reads=[YA[bi], Esb[bi]], writes=[hb])
            kb.op('dve', lambda e: e.tensor_tensor(out=Fsb[bset][bi][:], in0=hv(0, 256), in1=AK[bi][:], op=ALU.add), reads=[hb, AK[bi]], writes=[Fsb[bset][bi]])

    def seq_step(p, bset, bi, sidx, ost, col):
        Scur, Snext = ST[p][sidx % 2], ST[p][(sidx + 1) % 2]
        hb, hv = half()
        kb.op('pe', lambda e: e.matmul(hv(0, 128), lhsT=VT[bset][bi][:], rhs=Fsb[bset][bi][:, 0:128], start=True, stop=False), reads=[VT[bset][bi], Fsb[bset][bi]], writes=[hb])
        kb.op('pe', lambda e: e.matmul(hv(0, 128), lhsT=Scur[:], rhs=Qc[bset][bi][:], start=False, stop=True), reads=[Scur, Qc[bset][bi]], writes=[hb])
        kb.op('dve', lambda e: e.tensor_reduce(out=ost[:, col:col + LCH], in_=hv(0, 128).rearrange("p (h t) -> p t h", h=2), axis=AX.X, op=ALU.add), reads=[hb], writes=[ost])
        hb, hv = half()
        kb.op('pe', lambda e: e.matmul(hv(0, 128), lhsT=Fsb[bset][bi][:, 128:256], rhs=VT[bset][bi][:], start=True, stop=False), reads=[VT[bset][bi], Fsb[bset][bi]], writes=[hb])
        kb.op('pe', lambda e: e.matmul(hv(0, 128), lhsT=Mc[bset][bi][:], rhs=Scur[:], start=False, stop=True), reads=[Scur, Mc[bset][bi]], writes=[hb])
        kb.op('act', lambda e: e.copy(out=Snext[:], in_=hv(0, 128)), reads=[hb], writes=[Snext])

    bctr = 0
    for d in range(2):
        order = [groups[0]] + (groups[1:] if d == 0 else groups[1:][::-1])
        sidx = [0, 0]
        for p in range(2):
            kb.op('pool', lambda e: e.memset(ST[p][0][:], 0.0), writes=[ST[p][0]])
        for grp in order:
            t0, ntok, isctx = grp
            nch = ntok // LCH
            chs = list(range(nch)) if d == 0 else list(range(nch))[::-1]
            for p in range(2):
                prep(d, grp, p, p)
            for b0 in range(0, nch, NB):
                batch = chs[b0:b0 + NB]
                sets = []
                for p in range(2):
                    bset = bctr % 2
                    bctr += 1
                    precompute(d, p, batch, bset)
                    sets.append(bset)
                for bi, ci in enumerate(batch):
                    for p in range(2):
                        seq_step(p, sets[p], bi, sidx[p], ostage[p], ci * LCH)
                        sidx[p] += 1
            for p in range(2):
                kb.op('pool', lambda e: e.dma_start(out=odir_d[d, p, :, t0:t0 + ntok], in_=ostage[p][:, 0:ntok]), reads=[ostage[p]], dma=True)

    kb.pop()
    kb.push()
    NS = 2
    rawS = [kb.ring(3, [128, 514], F32) for _ in range(NS)]
    cqS = [kb.ring(3, [128, 512], F32) for _ in range(NS)]
    aS = [kb.ring(2, [128, 512], F32) for _ in range(NS)]
    xadS = [kb.ring(2, [65, 512], F32) for _ in range(NS)]
    for xs_ in xadS:
        for x_ in xs_:
            kb.op('pool', lambda e: e.memset(x_[64:65, :], 1.0), writes=[x_])
    tS = [kb.ring(6, [128, 512], F32) for _ in range(NS)]
    obufS = [kb.ring(2, [128, 512], F32) for _ in range(NS)]
    xgS = kb.ring(NS, [128, 512], F32)
    ob16S = kb.ring(NS, [128, 512], BF16)
    it = 0
    for grp in groups:
        t0, ntok, isctx = grp
        if isctx and not with_ctx:
            continue
        N = slice(0, ntok)
        for p in range(2):
            si = it % NS
            it += 1
            cq_, a_s, xad_, obuf, xg_t, o16 = cqS[si], aS[si], xadS[si], obufS[si], xgS[si], ob16S[si]
            for q, j in enumerate((0 + p, 2 + p, 4 + p)):
                r_ = load_raw(q, j, t0, ntok, rawS[si])
                conv(cq_[q], r_, p * 9 + q * 3, ntok)
            k_, v_, r_c = cq_
            sig_a(a_s[0], xad_[0], 0, p, t0, ntok)
            sig_a(a_s[1], xad_[1], 1, p, t0, ntok)
            kk, t1, kkn, u_, bop, t2 = tS[si]
            kb.op('dve', lambda e: e.tensor_tensor(out=u_[:, N], in0=a_s[0][:, N], in1=a_s[1][:, N], op=ALU.add), reads=[a_s[0], a_s[1]], writes=[u_])
            kb.op('dve', lambda e: e.tensor_scalar(out=u_[:, N], in0=u_[:, N], scalar1=dv[:, p, 1:2], scalar2=dv[:, p, 0:1], op0=ALU.mult, op1=ALU.add), reads=[u_, dv], writes=[u_])
            kb.op('pool', lambda e: e.tensor_tensor(out=u_[:, N], in0=k_[:, N], in1=u_[:, N], op=ALU.mult), reads=[k_, u_], writes=[u_])
            kb.op('dve', lambda e: e.scalar_tensor_tensor(out=t1[:, N], in0=r_c[:, N], scalar=cv[:, p * 5 + 2:p * 5 + 3], in1=u_[:, N], op0=ALU.mult, op1=ALU.mult), reads=[r_c, cv, u_], writes=[t1])
            hb0, hv0 = half()
            kb.op('pe', lambda e: e.matmul(hv0(0, ntok), lhsT=BONES, rhs=t1[:, N], start=True, stop=True), reads=[RC, t1], writes=[hb0])
            kb.op('dve', lambda e: e.tensor_tensor(out=bop[:, N], in0=hv0(0, ntok), in1=v_[:, N], op=ALU.mult), reads=[hb0, v_], writes=[bop])
            for dd_ in range(2):
                kb.op('sp', lambda e: e.dma_start(out=obuf[dd_][:, N], in_=odir_d[dd_, p, :, t0:t0 + ntok]), writes=[obuf[dd_]], dma=True)
            kb.op('pool', lambda e: e.tensor_tensor(out=kk[:, N], in0=obuf[0][:, N], in1=obuf[1][:, N], op=ALU.add), reads=[obuf[0], obuf[1]], writes=[kk])
            hb1, hv1 = half()
            kb.op('pe', lambda e: e.matmul(hv1(0, ntok), lhsT=BONES64, rhs=kk[:, N], start=True, stop=True), reads=[RC, kk], writes=[hb1])
            kb.op('dve', lambda e: e.tensor_tensor(out=kkn[:, N], in0=kk[:, N], in1=hv1(0, ntok), op=ALU.subtract), reads=[kk, hb1], writes=[kkn])
            kb.op('pool', lambda e: e.tensor_tensor(out=t1[:, N], in0=kkn[:, N], in1=kkn[:, N], op=ALU.mult), reads=[kkn], writes=[t1])
            hb2, hv2 = half()
            kb.op('pe', lambda e: e.matmul(hv2(0, ntok), lhsT=BONES64, rhs=t1[:, N], start=True, stop=True), reads=[RC, t1], writes=[hb2])
            kb.op('dve', lambda e: e.tensor_scalar(out=t2[:, N], in0=hv2(0, ntok), scalar1=GN_EPS, scalar2=None, op0=ALU.add), reads=[hb2], writes=[t2])
            kb.op('act', lambda e: e.activation(out=t2[:, N], in_=t2[:, N], func=AF.Sqrt), reads=[t2], writes=[t2])
            kb.op('dve', lambda e: e.reciprocal(out=t2[:, N], in_=t2[:, N]), reads=[t2], writes=[t2])
            kb.op('pool', lambda e: e.tensor_tensor(out=kkn[:, N], in0=kkn[:, N], in1=t2[:, N], op=ALU.mult), reads=[kkn, t2], writes=[kkn])
            kb.op('dve', lambda e: e.tensor_scalar(out=kkn[:, N], in0=kkn[:, N], scalar1=cv[:, p * 5 + 3:p * 5 + 4], scalar2=cv[:, p * 5 + 4:p * 5 + 5], op0=ALU.mult, op1=ALU.add), reads=[kkn, cv], writes=[kkn])
            kb.op('dve', lambda e: e.tensor_tensor(out=kkn[:, N], in0=kkn[:, N], in1=bop[:, N], op=ALU.add), reads=[kkn, bop], writes=[kkn])
            kb.op('sp', lambda e: e.dma_start(out=xg_t[:, N], in_=rwp_d[8, :, t0:t0 + ntok]), writes=[xg_t], dma=True)
            kb.op('act', lambda e: e.activation(out=xg_t[:, N], in_=xg_t[:, N], func=AF.Sigmoid), reads=[xg_t], writes=[xg_t])
            hb3, hv3 = half()
            kb.op('pe', lambda e: e.matmul(hv3(0, ntok), lhsT=gup[:, p * 128:(p + 1) * 128], rhs=xg_t[:, N], start=True, stop=True), reads=[gup, xg_t], writes=[hb3])
            kb.op('dve', lambda e: e.tensor_tensor(out=o16[:, N], in0=kkn[:, N], in1=hv3(0, ntok), op=ALU.mult), reads=[kkn, hb3], writes=[o16])
            dst, tl = (orwc_d, t0) if isctx else (orw_d, t0 - CTX)
            for hh_ in range(2):
                kb.op('pool', lambda e: e.dma_start(out=dst[2 * p + hh_, :, tl:tl + ntok], in_=o16[hh_ * 64:(hh_ + 1) * 64, N]), reads=[o16], dma=True)
    kb.pop()
    kb.pop()


def rwkv_inputs(l, hh, inp):
    ch = slice(hh * 256, (hh + 1) * 256)
    cwv = inp['conv_w'][l]
    cwc = np.zeros((128, 18), np.float32)
    for p in range(2):
        for q, base in enumerate((512, 1024, 0)):
            cols = slice(base + hh * 256 + p * 128, base + hh * 256 + (p + 1) * 128)
            cwc[:, p * 9 + q * 3: p * 9 + q * 3 + 3] = cwv[:, cols].T
    wupA = np.concatenate([inp['w_up'][l][:, :, ch], inp['w0'][l][:, None, ch]], axis=1)
    aupA = np.concatenate([inp['a_up'][l][:, :, ch], inp['a0'][l][:, None, ch]], axis=1)
    vecs = [inp['k_k'][l][ch], inp['k_a'][l][ch], inp['r_k'][l].reshape(-1)[ch], inp['ln_x_g'][l][ch], inp['ln_x_b'][l][ch]]
    cvv = np.zeros((128, 10), np.float32)
    for p in range(2):
        for i, v in enumerate(vecs):
            cvv[:, p * 5 + i] = v[p * 128:(p + 1) * 128]
    return {
        "rw_cw": cwc, "rw_wupA": np.ascontiguousarray(wupA), "rw_aupA": np.ascontiguousarray(aupA),
        "rw_gup": np.ascontiguousarray(inp['g_up'][l][:, ch]), "rw_cv": cvv, "rconsts": make_rconsts(),
    }


SW_LIMIT = 7.0
SW_ALPHA = 1.702
GMAX = 9


def ffn_groups(ntile):
    ng = (ntile + GMAX - 1) // GMAX
    base, rem = ntile // ng, ntile % ng
    out, t = [], 0
    for g in range(ng):
        n = base + (1 if g < rem else 0)
        out.append((t, n))
        t += n
    return out


def build_ffn(T, last, debug=False, n_exp=32, kb=None, ntiles=None, ctx_tiles=None, select=None, oT_attn_half=None):
    own = kb is None
    if own:
        kb = KB()
    nc = kb.nc
    kb.push()
    NLT = T // 256
    NT2 = (NLT + (0 if last else 1)) if ntiles is None else ntiles
    if ctx_tiles is None:
        ctx_tiles = set() if last else {NT2 - 1}
    NTL = NT2 * 128
    oT_d = kb.dram("oT", [16, 64, NTL], BF16, "ExternalInput")
    xres_d = kb.dram("xres", [NTL, D], F32, "ExternalInput")
    cc_d = kb.dram("cc", [2, D], F32, "ExternalInput")
    wmod_d = kb.dram("w_mod", [D, 6 * D], F32, "ExternalInput")
    bmod2_d = kb.dram("b_mod2", [2, 6 * D], F32, "ExternalInput")
    g2a_d = kb.dram("gmix2", [2, D], F32, "ExternalInput")
    g2b_d = kb.dram("gffn2", [2, D], F32, "ExternalInput")
    cst = kb.dram("consts", [128, C_W], F32, "ExternalInput")
    wout_d = kb.dram("w_out", [D, D], F32, "ExternalInput")
    rw_d = kb.dram("router_w", [D, 32], F32, "ExternalInput")
    rb_d = kb.dram("router_b128", [128, 32], F32, "ExternalInput")
    w1_d = kb.dram("e_w1", [32, D, 2 * D], F32, "ExternalInput")
    b1_d = kb.dram("e_b1T", [32, 128, 16], F32, "ExternalInput")
    w2_d = kb.dram("e_w2", [32, D, D], F32, "ExternalInput")
    b2_d = kb.dram("e_b2", [32, D], F32, "ExternalInput")
    gfin_d = kb.dram("gfin128", [128, D], F32, "ExternalInput")
    xout_d = kb.dram("xout", [NTL, D], F32, "ExternalOutput")
    xmid_d = kb.dram("xmid", [NTL, D], F32, "ExternalOutput" if debug else "Internal")

    identF = kb.sb([128, 128], F32)
    kb.op('sp', lambda e: e.dma_start(out=identF[:], in_=cst[:, C_ID:C_ID + 128]), writes=[identF], dma=True)
    if select is not None:
        msel_d = kb.dram("msel", [128, 2], F32, "ExternalInput")
        msel = kb.sb([128, 2], F32)
        kb.op('sp', lambda e: e.dma_start(out=msel[:], in_=msel_d), writes=[msel], dma=True)
    seven = kb.sb([128, 1], F32)
    kb.op('pool', lambda e: e.memset(seven[:], SW_LIMIT), writes=[seven])
    _, fm, bc = phase_mod(kb, cst, identF, cc_d, wmod_d, bmod2_d, g2a_d, g2b_d, True)
    B = kb.banks
    Wr = kb.sb([128, 8, 32], F32)
    kb.op('sp', lambda e: e.dma_start(out=Wr[:], in_=rw_d.rearrange("(k p) c -> p k c", p=128)), writes=[Wr], dma=True)
    rb = kb.sb([128, 32], F32)
    kb.op('sp', lambda e: e.dma_start(out=rb[:], in_=rb_d), writes=[rb], dma=True)
    b2sb = kb.sb([32, D], F32)
    kb.op('sp', lambda e: e.dma_start(out=b2sb[:], in_=b2_d), writes=[b2sb], dma=True)
    b1sb = kb.sb([128, 32, 16], F32)
    kb.op('sp', lambda e: e.dma_start(out=b1sb[:], in_=b1_d.rearrange("e p j -> p e j")), writes=[b1sb], dma=True)
    hT = kb.sb([128, 8, GMAX * 128], BF16)
    G = kb.sb([128, GMAX, 32], F32)
    GT = kb.sb([32, GMAX, 128], F32)
    w1src = w1_d.rearrange("e (k p) c -> e p k c", p=128)
    w2src = w2_d.rearrange("e (k p) c -> e p k c", p=128)

    for (g0, gn) in ffn_groups(NT2):
        gtok = gn * 128
        kb.push()
        Wo = kb.sb([64, 16, D], BF16)
        wosrc = wout_d.rearrange("(h r) c -> r h c", r=64)
        for h4 in range(4):
            kb.op('pool', lambda e: e.dma_start(out=Wo[:, h4 * 4:(h4 + 1) * 4, :], in_=wosrc[:, h4 * 4:(h4 + 1) * 4, :]), writes=[Wo], dma=True)
        oT_ring = kb.ring(2, [64, 16, 128], BF16)
        x_ring = kb.ring(2, [128, D], F32)
        if select is not None:
            oTb_ring = kb.ring(2, [64, 16, 128], BF16)
            xb_ring = kb.ring(2, [128, D], F32)
        x1_ring = kb.ring(2, [128, D], F32)
        xn_ring = kb.ring(2, [128, D], F32)
        junk = kb.sb([128, D], BF16)
        st_ring = kb.ring(2, [128, 24], F32)
        h32_ring = kb.ring(2, [128, 8, 128], F32)
        lg_ring = kb.ring(2, [128, 3, 32], F32)
        oTsrc = oT_d.rearrange("h r t -> r h t")
        for i in range(gn):
            ti = g0 + i
            r = 1 if ti in ctx_tiles else 0
            rows = slice(ti * 128, (ti + 1) * 128)
            ot, xt, x1, xn, st, h32, lg = oT_ring[i % 2], x_ring[i % 2], x1_ring[i % 2], xn_ring[i % 2], st_ring[i % 2], h32_ring[i % 2], lg_ring[i % 2]
            if select is None:
                kb.op('sp', lambda e: e.dma_start(out=ot[:], in_=oTsrc[:, :, ti * 128:(ti + 1) * 128]), writes=[ot], dma=True)
                kb.op('sp', lambda e: e.dma_start(out=xt[:], in_=xres_d[rows, :]), writes=[xt], dma=True)
            else:
                ca, cb = (select[0] + ti) * 128, (select[1] + ti) * 128
                otb, xtb = oTb_ring[i % 2], xb_ring[i % 2]
                if oT_attn_half is None:
                    s0 = 0
                    kb.op('sp', lambda e: e.dma_start(out=ot[:], in_=oTsrc[:, :, ca:ca + 128]), writes=[ot], dma=True)
                    kb.op('sp', lambda e: e.dma_start(out=otb[:], in_=oTsrc[:, :, cb:cb + 128]), writes=[otb], dma=True)
                else:
                    s0 = 8
                    kb.op('sp', lambda e: e.dma_start(out=ot[:, 0:8, :], in_=oT_attn_half.rearrange("h r t -> r h t")[:, :, ti * 128:(ti + 1) * 128]), writes=[ot], dma=True)
                    kb.op('sp', lambda e: e.dma_start(out=ot[:, 8:16, :], in_=oTsrc[:, 8:16, ca:ca + 128]), writes=[ot], dma=True)
                    kb.op('sp', lambda e: e.dma_start(out=otb[:, 8:16, :], in_=oTsrc[:, 8:16, cb:cb + 128]), writes=[otb], dma=True)
                kb.op('sp', lambda e: e.dma_start(out=xt[:], in_=xres_d[ca:ca + 128, :]), writes=[xt], dma=True)
                kb.op('sp', lambda e: e.dma_start(out=xtb[:], in_=xres_d[cb:cb + 128, :]), writes=[xtb], dma=True)
                o2 = lambda t: t[:, s0:16, :].rearrange("p h t -> p (h t)")
                kb.op('dve', lambda e: e.tensor_scalar(out=o2(ot), in0=o2(ot), scalar1=msel[0:64, 0:1], scalar2=None, op0=ALU.mult), reads=[ot, msel], writes=[ot])
                kb.op('dve', lambda e: e.scalar_tensor_tensor(out=o2(ot), in0=o2(otb), scalar=msel[0:64, 1:2], in1=o2(ot), op0=ALU.mult, op1=ALU.add), reads=[ot, otb, msel], writes=[ot])
                kb.op('dve', lambda e: e.tensor_scalar(out=xt[:], in0=xt[:], scalar1=msel[:, 0:1], scalar2=None, op0=ALU.mult), reads=[xt, msel], writes=[xt])
                kb.op('dve', lambda e: e.scalar_tensor_tensor(out=xt[:], in0=xtb[:], scalar=msel[:, 1:2], in1=xt[:], op0=ALU.mult, op1=ALU.add), reads=[xt, xtb, msel], writes=[xt])
            for hf in range(2):
                for h in range(16):
                    kb.op('pe', lambda e: e.matmul(B[hf][:, 0:512], lhsT=ot[:, h, :], rhs=Wo[:, h, hf * 512:(hf + 1) * 512], start=(h == 0), stop=(h == 15)),
                          reads=[ot, Wo], writes=[B[hf]])
                kb.op('dve', lambda e: e.tensor_tensor(out=x1[:, hf * 512:(hf + 1) * 512], in0=B[hf][:, 0:512], in1=bc[('gm', r)][:, hf * 512:(hf + 1) * 512], op=ALU.mult),
                      reads=[B[hf], bc[('gm', r)]], writes=[x1])
            kb.op('pool', lambda e: e.tensor_tensor(out=x1[:], in0=x1[:], in1=xt[:], op=ALU.add), reads=[x1, xt], writes=[x1])
            kb.op('pool', lambda e: e.dma_start(out=xmid_d[rows, :], in_=x1[:]), reads=[x1], dma=True)
            kb.op('act', lambda e: e.activation(out=junk[:], in_=x1[:], func=AF.Square, accum_out=st[:, 0:1]), reads=[x1], writes=[junk, st])
            kb.op('dve', lambda e: e.tensor_scalar(out=st[:, 1:2], in0=st[:, 0:1], scalar1=1.0 / D, scalar2=EPS, op0=ALU.mult, op1=ALU.add), reads=[st], writes=[st])
            kb.op('act', lambda e: e.activation(out=st[:, 1:2], in_=st[:, 1:2], func=AF.Sqrt), reads=[st], writes=[st])
            kb.op('dve', lambda e: e.reciprocal(out=st[:, 2:3], in_=st[:, 1:2]), reads=[st], writes=[st])
            kb.op('pool', lambda e: e.tensor_scalar(out=xn[:], in0=x1[:], scalar1=st[:, 2:3], scalar2=None, op0=ALU.mult), reads=[x1, st], writes=[xn])
            for k in range(8):
                bank = B[2 + k // 4]
                kb.op('pe', lambda e: e.transpose(out=bank[:, (k % 4) * 128:(k % 4 + 1) * 128], in_=xn[:, k * 128:(k + 1) * 128], identity=identF[:]), reads=[xn, identF], writes=[bank])
            for k in range(8):
                bank = B[2 + k // 4]
                kb.op('act', lambda e: e.activation(out=h32[:, k, :], in_=bank[:, (k % 4) * 128:(k % 4 + 1) * 128], func=AF.Identity, scale=fm[:, 2, k, r:r + 1], bias=fm[:, 3, k, r:r + 1]),
                      reads=[bank, fm], writes=[h32])
            kb.op('pool', lambda e: e.tensor_copy(out=hT[:, :, i * 128:(i + 1) * 128], in_=h32[:]), reads=[h32], writes=[hT])
            for k in range(8):
                kb.op('pe', lambda e: e.matmul(B[4][:, 0:32], lhsT=h32[:, k, :], rhs=Wr[:, k, :], start=(k == 0), stop=(k == 7)), reads=[h32, Wr], writes=[B[4]])
            kb.op('dve', lambda e: e.tensor_tensor(out=lg[:, 0, :], in0=B[4][:, 0:32], in1=rb[:], op=ALU.add), reads=[B[4], rb], writes=[lg])
            kb.op('dve', lambda e: e.max(out=st[:, 8:16], in_=lg[:, 0, :]), reads=[lg], writes=[st])
            kb.op('dve', lambda e: e.tensor_scalar(out=st[:, 16:17], in0=st[:, 8:9], scalar1=-1.0, scalar2=None, op0=ALU.mult), reads=[st], writes=[st])
            kb.op('dve', lambda e: e.tensor_scalar(out=lg[:, 1, :], in0=lg[:, 0, :], scalar1=st[:, 11:12], scalar2=None, op0=ALU.is_ge), reads=[lg, st], writes=[lg])
            kb.op('act', lambda e: e.activation(out=lg[:, 2, :], in_=lg[:, 0, :], func=AF.Exp, bias=st[:, 16:17], scale=1.0), reads=[lg, st], writes=[lg])
            kb.op('dve', lambda e: e.tensor_tensor(out=lg[:, 2, :], in0=lg[:, 2, :], in1=lg[:, 1, :], op=ALU.mult), reads=[lg], writes=[lg])
            kb.op('dve', lambda e: e.tensor_reduce(out=st[:, 17:18], in_=lg[:, 2, :], axis=AX.X, op=ALU.add), reads=[lg], writes=[st])
            kb.op('dve', lambda e: e.reciprocal(out=st[:, 18:19], in_=st[:, 17:18]), reads=[st], writes=[st])
            kb.op('dve', lambda e: e.tensor_scalar(out=G[:, i, :], in0=lg[:, 2, :], scalar1=st[:, 18:19], scalar2=None, op0=ALU.mult), reads=[lg, st], writes=[G])
            kb.op('pe', lambda e: e.transpose(out=B[5][0:32, 0:128], in_=G[:, i, :], identity=identF[:]), reads=[G, identF], writes=[B[5]])
            kb.op('act', lambda e: e.copy(out=GT[:, i, :], in_=B[5][0:32, 0:128]), reads=[B[5]], writes=[GT])
        kb.pop()
        kb.push()
        yacc = kb.sb([128, gn, D], F32)
        kb.push()
        W1 = kb.ring(2, [128, 8, 2 * D], BF16)
        W2 = kb.ring(2, [128, 8, D], BF16)
        actT = kb.ring(2, [128, 8, 512], BF16)
        g1r = kb.ring(2, [128, 512], F32)
        sgr = kb.ring(2, [128, 512], BF16)
        l1r = kb.ring(2, [128, 512], F32)
        chunks = []
        c0 = 0
        while c0 < gtok:
            n = min(512, gtok - c0)
            chunks.append((c0, n))
            c0 += n
        jobs = [(e, c) for e in range(n_exp) for c in chunks]

        def load_w(e):
            w1, w2 = W1[e % 2], W2[e % 2]
            for k in range(8):
                kb.op('pool', lambda e_: e_.dma_start(out=w1[:, k, :], in_=w1src[e, :, k, :]), writes=[w1], dma=True)
            for k4 in range(2):
                kb.op('pool', lambda e_: e_.dma_start(out=w2[:, k4 * 4:(k4 + 1) * 4, :], in_=w2src[e, :, k4 * 4:(k4 + 1) * 4, :]), writes=[w2], dma=True)

        ucnt = [0]

        def emit_U(n):
            e, (c0, cn) = jobs[n]
            w1 = W1[e % 2]
            at = actT[n % 2]
            for j in range(8):
                u = ucnt[0]
                ucnt[0] += 1
                bg, bl = B[(u % 2) * 2], B[(u % 2) * 2 + 1]
                g1, sg_, l1 = g1r[u % 2], sgr[u % 2], l1r[u % 2]
                for k in range(8):
                    kb.op('pe', lambda e_: e_.matmul(bg[:, 0:cn], lhsT=w1[:, k, j * 128:(j + 1) * 128], rhs=hT[:, k, c0:c0 + cn], start=(k == 0), stop=(k == 7)), reads=[w1, hT], writes=[bg])
                for k in range(8):
                    kb.op('pe', lambda e_: e_.matmul(bl[:, 0:cn], lhsT=w1[:, k, D + j * 128:D + (j + 1) * 128], rhs=hT[:, k, c0:c0 + cn], start=(k == 0), stop=(k == 7)), reads=[w1, hT], writes=[bl])
                kb.op('dve', lambda e_: e_.tensor_scalar(out=g1[:, 0:cn], in0=bg[:, 0:cn], scalar1=b1sb[:, e, j:j + 1], scalar2=SW_LIMIT, op0=ALU.add, op1=ALU.min), reads=[bg, b1sb], writes=[g1])
                kb.op('act', lambda e_: e_.activation(out=sg_[:, 0:cn], in_=g1[:, 0:cn], func=AF.Sigmoid, scale=SW_ALPHA), reads=[g1], writes=[sg_])
                kb.op('dve', lambda e_: e_.tensor_scalar(out=l1[:, 0:cn], in0=bl[:, 0:cn], scalar1=b1sb[:, e, 8 + j:9 + j], scalar2=SW_LIMIT, op0=ALU.add, op1=ALU.min), reads=[bl, b1sb], writes=[l1])
                kb.op('act', lambda e_: e_.activation(out=l1[:, 0:cn], in_=l1[:, 0:cn], func=AF.Relu, bias=seven[:, 0:1], scale=1.0), reads=[l1, seven], writes=[l1])
                kb.op('dve', lambda e_: e_.tensor_tensor(out=g1[:, 0:cn], in0=g1[:, 0:cn], in1=sg_[:, 0:cn], op=ALU.mult), reads=[g1, sg_], writes=[g1])
                kb.op('dve', lambda e_: e_.scalar_tensor_tensor(out=at[:, j, 0:cn], in0=l1[:, 0:cn], scalar=1.0 - SW_LIMIT, in1=g1[:, 0:cn], op0=ALU.add, op1=ALU.mult), reads=[g1, l1], writes=[at])

        wcnt = [0]

        def emit_W2(n):
            e, (c0, cn) = jobs[n]
            w2 = W2[e % 2]
            at = actT[n % 2]
            for tt in range(cn // 128):
                ti = (c0 // 128) + tt
                for hf in range(2):
                    w = wcnt[0]
                    wcnt[0] += 1
                    bank = B[4 + w % 4]
                    for j in range(8):
                        kb.op('pe', lambda e_: e_.matmul(bank[:, 0:512], lhsT=at[:, j, tt * 128:(tt + 1) * 128], rhs=w2[:, j, hf * 512:(hf + 1) * 512], start=(j == 0), stop=(j == 7)), reads=[at, w2], writes=[bank])
                    ya = yacc[:, ti, hf * 512:(hf + 1) * 512]
                    if e == 0:
                        kb.op('dve', lambda e_: e_.tensor_scalar(out=ya, in0=bank[:, 0:512], scalar1=G[:, ti, e:e + 1], scalar2=None, op0=ALU.mult), reads=[bank, G], writes=[yacc])
                    else:
                        kb.op('dve', lambda e_: e_.scalar_tensor_tensor(out=ya, in0=bank[:, 0:512], scalar=G[:, ti, e:e + 1], in1=ya, op0=ALU.mult, op1=ALU.add), reads=[bank, G, yacc], writes=[yacc])

        load_w(0)
        if n_exp > 1:
            load_w(1)
        emit_U(0)
        for n in range(len(jobs)):
            if n + 1 < len(jobs):
                emit_U(n + 1)
            emit_W2(n)
            e, c = jobs[n]
            if c == chunks[-1] and e + 2 < n_exp:
                load_w(e + 2)
        kb.pop()
        x_ring = kb.ring(2, [128, D], F32)
        gfin = kb.sb([128, D], F32)
        stf = kb.ring(2, [128, 4], F32)
        junk2 = kb.sb([128, D], BF16)
        if last:
            kb.op('sp', lambda e: e.dma_start(out=gfin[:], in_=gfin_d), writes=[gfin], dma=True)
        for i in range(gn):
            ti = g0 + i
            r = 1 if ti in ctx_tiles else 0
            rows = slice(ti * 128, (ti + 1) * 128)
            xt = x_ring[i % 2]
            kb.op('sp', lambda e: e.dma_start(out=xt[:], in_=xmid_d[rows, :]), writes=[xt], dma=True)
            for hf in range(2):
                kb.op('pe', lambda e: e.matmul(B[hf][:, 0:512], lhsT=GT[:, i, :], rhs=b2sb[:, hf * 512:(hf + 1) * 512], start=True, stop=True), reads=[GT, b2sb], writes=[B[hf]])
                ya = yacc[:, i, hf * 512:(hf + 1) * 512]
                kb.op('dve', lambda e: e.tensor_tensor(out=ya, in0=B[hf][:, 0:512], in1=ya, op=ALU.add), reads=[B[hf], yacc], writes=[yacc])
            kb.op('pool', lambda e: e.tensor_tensor(out=yacc[:, i, :], in0=yacc[:, i, :], in1=bc[('gf', r)][:], op=ALU.mult), reads=[yacc, bc[('gf', r)]], writes=[yacc])
            kb.op('pool', lambda e: e.tensor_tensor(out=xt[:], in0=xt[:], in1=yacc[:, i, :], op=ALU.add), reads=[xt, yacc], writes=[xt])
            if last:
                st = stf[i % 2]
                kb.op('act', lambda e: e.activation(out=junk2[:], in_=xt[:], func=AF.Square, accum_out=st[:, 0:1]), reads=[xt], writes=[junk2, st])
                kb.op('dve', lambda e: e.tensor_scalar(out=st[:, 1:2], in0=st[:, 0:1], scalar1=1.0 / D, scalar2=EPS, op0=ALU.mult, op1=ALU.add), reads=[st], writes=[st])
                kb.op('act', lambda e: e.activation(out=st[:, 1:2], in_=st[:, 1:2], func=AF.Sqrt), reads=[st], writes=[st])
                kb.op('dve', lambda e: e.reciprocal(out=st[:, 2:3], in_=st[:, 1:2]), reads=[st], writes=[st])
                kb.op('dve', lambda e: e.scalar_tensor_tensor(out=xt[:], in0=xt[:], scalar=st[:, 2:3], in1=gfin[:], op0=ALU.mult, op1=ALU.mult), reads=[xt, st, gfin], writes=[xt])
            kb.op('pool', lambda e: e.dma_start(out=xout_d[rows, :], in_=xt[:]), reads=[xt], dma=True)
        kb.pop()
    kb.pop()
    if own:
        kb.finish()
    return nc


def ffn_inputs(T, l, b, hh, last, oT_core, xres_core, inp):
    return {
        "oT": oT_core, "xres": xres_core,
        "cc": np.ascontiguousarray(np.stack([inp['c'][b], inp['c_ctx']])),
        "w_mod": np.ascontiguousarray(inp['w_mod'][l]),
        "b_mod2": np.ascontiguousarray(np.tile(inp['b_mod'][l][None], (2, 1))),
        "gmix2": np.ascontiguousarray(np.tile(inp['norm_mix_g'][l][None], (2, 1))),
        "gffn2": np.ascontiguousarray(np.tile(inp['norm_ffn_g'][l][None], (2, 1))),
        "consts": make_consts(),
        "w_out": np.ascontiguousarray(inp['w_out'][l]),
        "router_w": np.ascontiguousarray(inp['router_w'][l]),
        "router_b128": np.ascontiguousarray(np.tile(inp['router_b'][l][None], (128, 1))),
        "e_w1": np.ascontiguousarray(inp['e_w1'][l]),
        "e_b1T": np.ascontiguousarray(inp['e_b1'][l].reshape(32, 16, 128).transpose(0, 2, 1)),
        "e_w2": np.ascontiguousarray(inp['e_w2'][l]),
        "e_b2": np.ascontiguousarray(inp['e_b2'][l]),
        "gfin128": np.ascontiguousarray(np.tile(inp['norm_final_g'][None], (128, 1))),
    }


MIX_HH = ("w_in_c", "rw_cw", "rw_wupA", "rw_aupA", "rw_gup", "rw_cv")
LAYER_NAMES = ("w_mod", "b_mod2", "gmix2", "gffn2", "gains320", "w_out", "router_w", "router_b128", "e_w1", "e_b1T", "e_w2", "e_b2")


def build_fused(T, n_exp=32, debug=False):
    kb = KB()
    nc = kb.nc
    NTOK = CTX + T
    NT = NTOK // 128
    H = T // 2
    xin0 = kb.dram("xin", [NTOK, D], F32, "ExternalInput")
    oall = kb.dram("oall", [16, 64, NTOK], BF16, "Internal")
    xl1 = kb.dram("xl1", [NTOK, D], F32, "ExternalOutput" if debug else "Internal")
    xmid0 = kb.dram("xmid0", [NTOK, D], F32, "Internal")
    xmid1 = kb.dram("xmid1", [H, D], F32, "Internal")
    xfin = kb.dram("xfin", [H, D], F32, "ExternalOutput")
    oallh = kb.dram("oallh", [8, 64, H], BF16, "Internal")
    rwp = kb.dram("rwproj", [9, 128, NTOK], F32, "Internal")
    odir = kb.dram("odir", [2, 2, 128, NTOK], F32, "Internal")
    for l in range(2):
        last = (l == 1)
        xin = xin0 if l == 0 else xl1
        for hh in range(2):
            kb.sfx = {n: f"_l{l}" for n in LAYER_NAMES}
            kb.sfx.update({n: f"_l{l}h{hh}" for n in MIX_HH})
            kb.override = {
                "xin": xin, "rwproj": rwp, "odir": odir,
                "oat": oall[hh * 4:(hh + 1) * 4, :, CTX:NTOK], "oatc": oall[hh * 4:(hh + 1) * 4, :, 0:CTX],
                "orw": oall[8 + hh * 4:8 + (hh + 1) * 4, :, CTX:NTOK], "orwc": oall[8 + hh * 4:8 + (hh + 1) * 4, :, 0:CTX],
            }
            if last:
                kb.override["oat"] = oallh[hh * 4:(hh + 1) * 4]
            build_mix(T, not last, kb=kb, q_half=last)
            kb.barrier()
        kb.sfx = {n: f"_l{l}" for n in LAYER_NAMES}
        if not last:
            kb.override = {"oT": oall, "xres": xin, "xout": xl1, "xmid": xmid0}
            build_ffn(T, False, n_exp=n_exp, kb=kb, ntiles=NT, ctx_tiles={0, 1})
        else:
            kb.override = {"oT": oall, "xres": xin, "xout": xfin, "xmid": xmid1}
            build_ffn(T, True, n_exp=n_exp, kb=kb, ntiles=H // 128, ctx_tiles=set(), select=(2, 2 + H // 128), oT_attn_half=oallh)
        kb.barrier()
    kb.finish()
    return nc


def fused_inputs(T, b, hh, inp):
    xl, xc = inp['x'], inp['ctx']
    cos, sin = rope_tables(T)
    m = {
        "xin": np.ascontiguousarray(np.concatenate([xc[b], xl[b]], axis=0)),
        "cc": np.ascontiguousarray(np.stack([inp['c'][b], inp['c_ctx']])),
        "consts": make_consts(), "rconsts": make_rconsts(), "rope_cos": cos, "rope_sin": sin,
        "gfin128": np.ascontiguousarray(np.tile(inp['norm_final_g'][None], (128, 1))),
        "msel": np.ascontiguousarray(np.tile(np.eye(2, dtype=np.float32)[hh][None], (128, 1))),
    }
    for l in range(2):
        gains = np.concatenate([np.tile(inp['q_norm_g'][l], 4), inp['k_norm_g'][l]])
        lay = {
            "w_mod": inp['w_mod'][l], "b_mod2": np.tile(inp['b_mod'][l][None], (2, 1)),
            "gmix2": np.tile(inp['norm_mix_g'][l][None], (2, 1)), "gffn2": np.tile(inp['norm_ffn_g'][l][None], (2, 1)),
            "gains320": np.tile(gains[None], (128, 1)), "w_out": inp['w_out'][l], "router_w": inp['router_w'][l],
            "router_b128": np.tile(inp['router_b'][l][None], (128, 1)), "e_w1": inp['e_w1'][l],
            "e_b1T": inp['e_b1'][l].reshape(32, 16, 128).transpose(0, 2, 1), "e_w2": inp['e_w2'][l], "e_b2": inp['e_b2'][l],
        }
        for k, v in lay.items():
            m[f"{k}_l{l}"] = np.ascontiguousarray(v, dtype=np.float32)
        w_in = inp['w_in'][l]
        sl = lambda a, n: w_in[:, a:a + n]
        for h2 in range(2):
            w_in_c = np.concatenate([
                sl(1536 + h2 * 256, 256), sl(h2 * 64, 64), sl(128 + h2 * 64, 64),
                sl(256 + h2 * 256, 256), sl(768 + h2 * 256, 256), sl(2048 + h2 * 256, 256),
                sl(1280, 128), sl(1408, 128), sl(2560, 128)], axis=1)
            m[f"w_in_c_l{l}h{h2}"] = np.ascontiguousarray(w_in_c)
            for k, v in rwkv_inputs(l, h2, inp).items():
                if k != "rconsts":
                    m[f"{k}_l{l}h{h2}"] = v
    return m


_NC_CACHE = {}


def run_fused(inp, T, n_exp=32):
    inp = {k: np.asarray(v) for k, v in inp.items()}
    Bn = inp['x'].shape[0]
    H = T // 2
    key = ('fused', T, n_exp)
    if key not in _NC_CACHE:
        _NC_CACHE[key] = build_fused(T, n_exp)
    cores = list(range(2 * Bn))
    res = run_bass_kernel_spmd(_NC_CACHE[key], [fused_inputs(T, c // 2, c % 2, inp) for c in cores], core_ids=cores).results
    out = np.empty((Bn, T, D), np.float32)
    for c in cores:
        out[c // 2, (c % 2) * H:(c % 2 + 1) * H] = np.asarray(res[c]['xfin'])
    return out


def kernel(**inputs):
    return run_fused(inputs, 8192)
```
